# Optimizing a Trainium2 kernel written in Bass

```python
import math
import jax, jax.numpy as jnp
from jax import lax
import numpy as np

D_MODEL = 2048
BATCH = 2
SEQ = 8192
DEPTH = 4

N_MIXERS = 2
GRID_W = 64
NA_HEADS = 16
NA_HEAD_DIM = D_MODEL // NA_HEADS
WIN_H = 8
WIN_W = 16
ML_PROJ_FACTOR = 2
ML_INNER = ML_PROJ_FACTOR * D_MODEL
ML_HEADS = 4
ML_HEAD_DIM = ML_INNER // ML_HEADS
ML_QKV_BLOCK = 4
ML_CONV_K = 4
ML_CHUNK = 64
MLP_HIDDEN = 4 * D_MODEL
RMS_EPS = 1e-6
LN_EPS = 1e-5

kernel_name = 'hybrid_natten_mlstm_encoder'


def rms_norm(x, g):
    xf = x.astype(jnp.float32)
    y = xf * lax.rsqrt(jnp.mean(xf * xf, axis=-1, keepdims=True) + RMS_EPS)
    return (y * g.astype(jnp.float32)).astype(x.dtype)


def neighbourhood_attention(h, w_qkv, q_gain, k_gain, rel_bias, w_o):
    B, T, _ = h.shape
    rows = T // GRID_W
    kh = min(WIN_H, rows)
    q, k, v = jnp.split(h @ w_qkv, 3, axis=-1)
    shp = (B, rows, GRID_W, NA_HEADS, NA_HEAD_DIM)
    q = rms_norm(q.reshape(shp), q_gain)
    k = rms_norm(k.reshape(shp), k_gain)
    v = v.reshape(shp)
    col_start = np.clip(np.arange(GRID_W) - WIN_W // 2, 0, GRID_W - WIN_W)
    col_idx = col_start[:, None] + np.arange(WIN_W)[None, :]
    rel_col = col_idx - np.arange(GRID_W)[:, None] + (WIN_W - 1)
    col_bias = rel_bias[:, :, rel_col]
    scale = NA_HEAD_DIM ** -0.5

    def row_block(r):
        r0 = jnp.clip(r - kh // 2, 0, rows - kh)
        q_r = lax.dynamic_index_in_dim(q, r, axis=1, keepdims=False)
        k_r = lax.dynamic_slice_in_dim(k, r0, kh, axis=1)[:, :, col_idx]
        v_r = lax.dynamic_slice_in_dim(v, r0, kh, axis=1)[:, :, col_idx]
        rel_row = r0 + jnp.arange(kh) - r + (WIN_H - 1)
        bias = jnp.take(col_bias, rel_row, axis=1).transpose(0, 2, 1, 3)
        s = jnp.einsum('bqhd,biqjhd->bhqij', q_r, k_r).astype(jnp.float32) * scale
        s = s + bias[None].astype(jnp.float32)
        p = jax.nn.softmax(s.reshape(B, NA_HEADS, GRID_W, kh * WIN_W), axis=-1)
        p = p.reshape(s.shape).astype(v.dtype)
        return jnp.einsum('bhqij,biqjhd->bqhd', p, v_r)

    out = lax.map(row_block, jnp.arange(rows))
    out = out.transpose(1, 0, 2, 3, 4).reshape(B, T, D_MODEL)
    return out @ w_o


def headwise(x, w):
    B, T, I = x.shape
    y = jnp.einsum('btgi,gio->btgo', x.reshape(B, T, I // ML_QKV_BLOCK, ML_QKV_BLOCK), w)
    return y.reshape(B, T, I)


def mlstm_chunkwise(q, k, v, i_pre, f_pre):
    B, H, T, Dh = q.shape
    L = ML_CHUNK
    nc = T // L
    k = k * (Dh ** -0.5)

    def to_chunks(a):
        return jnp.moveaxis(a.reshape((B, H, nc, L) + a.shape[3:]), 2, 0)

    qc, kc, vc = to_chunks(q), to_chunks(k), to_chunks(v)
    ic = to_chunks(i_pre)
    bc = jnp.cumsum(to_chunks(jax.nn.log_sigmoid(f_pre)), axis=-1)
    gc = bc[..., -1]
    tril = jnp.tril(jnp.ones((L, L), dtype=bool))

    def step(carry, xs):
        C, n, m = carry
        qj, kj, vj, ij, bj, gj = xs
        log_d = jnp.where(tril, bj[..., :, None] - bj[..., None, :] + ij[..., None, :], -jnp.inf)
        m_inter = bj + m[..., None]
        m_row = jnp.maximum(m_inter, jnp.max(log_d, axis=-1))
        s = jnp.einsum('bhld,bhsd->bhls', qj, kj) * jnp.exp(log_d - m_row[..., None])
        w_inter = jnp.exp(m_inter - m_row)
        num = jnp.einsum('bhls,bhsd->bhld', s, vj) + w_inter[..., None] * jnp.einsum('bhld,bhde->bhle', qj, C)
        den = jnp.sum(s, axis=-1) + w_inter * jnp.einsum('bhld,bhd->bhl', qj, n)
        h = num / jnp.maximum(jnp.abs(den), jnp.exp(-m_row))[..., None]
        log_w = gj[..., None] - bj + ij
        m_new = jnp.maximum(gj + m, jnp.max(log_w, axis=-1))
        wk = kj * jnp.exp(log_w - m_new[..., None])[..., None]
        decay = jnp.exp(gj + m - m_new)
        C_new = decay[..., None, None] * C + jnp.einsum('bhld,bhle->bhde', wk, vj)
        n_new = decay[..., None] * n + jnp.sum(wk, axis=2)
        return (C_new, n_new, m_new), h

    init = (jnp.zeros((B, H, Dh, Dh), jnp.float32), jnp.zeros((B, H, Dh), jnp.float32),
            jnp.zeros((B, H), jnp.float32))
    _, hs = lax.scan(step, init, (qc, kc, vc, ic, bc, gc))
    return jnp.moveaxis(hs, 0, 2).reshape(B, H, T, Dh)


def mlstm_layer(h, w_up, conv_w, conv_b, w_q, w_k, w_v, w_ig, b_ig, w_fg, b_fg, out_norm, skip, w_down):
    B, T, _ = h.shape
    I, NH, Dh = ML_INNER, ML_HEADS, ML_HEAD_DIM
    x_m, z = jnp.split(h @ w_up, 2, axis=-1)
    x_c = lax.conv_general_dilated(x_m, conv_w[:, None, :], window_strides=(1,),
                                   padding=[((ML_CONV_K - 1) // 2, ML_CONV_K // 2)],
                                   dimension_numbers=('NWC', 'WIO', 'NWC'),
                                   feature_group_count=I) + conv_b
    x_c = jax.nn.silu(x_c)
    q, k, v = headwise(x_c, w_q), headwise(x_c, w_k), headwise(x_m, w_v)

    def gate(w, b):
        return (q @ w[:I] + k @ w[I:2 * I] + v @ w[2 * I:] + b).astype(jnp.float32)

    i_pre, f_pre = gate(w_ig, b_ig), gate(w_fg, b_fg)
    heads = lambda a: a.astype(jnp.float32).reshape(B, T, NH, Dh).transpose(0, 2, 1, 3)
    qh, kh, vh = heads(q), heads(k), heads(v)
    gh = lambda a: a.transpose(0, 2, 1)
    h_f = mlstm_chunkwise(qh, kh, vh, gh(i_pre[..., :NH]), gh(f_pre[..., :NH]))
    fl = lambda a: jnp.flip(a, axis=2)
    h_b = fl(mlstm_chunkwise(fl(qh), fl(kh), fl(vh), jnp.flip(gh(i_pre[..., NH:]), -1),
                             jnp.flip(gh(f_pre[..., NH:]), -1)))
    hc = h_f + h_b
    mu = jnp.mean(hc, axis=-1, keepdims=True)
    var = jnp.mean(jnp.square(hc - mu), axis=-1, keepdims=True)
    hc = (hc - mu) * lax.rsqrt(var + LN_EPS)
    hc = hc.transpose(0, 2, 1, 3).reshape(B, T, I) * out_norm.astype(jnp.float32)
    y = (hc.astype(h.dtype) + skip * x_c) * jax.nn.silu(z)
    return y @ w_down


def sq_relu_mlp(h, w1, w2):
    a = jax.nn.relu(h @ w1)
    return (a * a) @ w2


def setup_inputs(seed: int = 0) -> dict:
    key = jax.random.key(seed)
    ks = iter(jax.random.split(key, 32))
    nrm = lambda shape, s: jax.random.normal(next(ks), shape, jnp.float32) * s
    n_a = (DEPTH + N_MIXERS - 1) // N_MIXERS
    n_m = DEPTH // N_MIXERS
    D, I, NH = D_MODEL, ML_INNER, ML_HEADS
    f_bias = jnp.tile(jnp.linspace(3.0, 6.0, NH, dtype=jnp.float32), 2)
    return {
        'x': nrm((BATCH, SEQ, D), 1.0),
        'norm_mix': 1.0 + nrm((DEPTH, D), 0.02),
        'norm_mlp': 1.0 + nrm((DEPTH, D), 0.02),
        'na_w_qkv': nrm((n_a, D, 3 * D), D ** -0.5),
        'na_q_gain': 1.0 + nrm((n_a, NA_HEAD_DIM), 0.02),
        'na_k_gain': 1.0 + nrm((n_a, NA_HEAD_DIM), 0.02),
        'na_rel_bias': nrm((n_a, NA_HEADS, 2 * WIN_H - 1, 2 * WIN_W - 1), 0.1),
        'na_w_o': nrm((n_a, D, D), D ** -0.5),
        'ml_w_up': nrm((n_m, D, 2 * I), D ** -0.5),
        'ml_conv_w': nrm((n_m, ML_CONV_K, I), ML_CONV_K ** -0.5),
        'ml_conv_b': nrm((n_m, I), 0.02),
        'ml_w_q': nrm((n_m, I // ML_QKV_BLOCK, ML_QKV_BLOCK, ML_QKV_BLOCK), ML_QKV_BLOCK ** -0.5),
        'ml_w_k': nrm((n_m, I // ML_QKV_BLOCK, ML_QKV_BLOCK, ML_QKV_BLOCK), ML_QKV_BLOCK ** -0.5),
        'ml_w_v': nrm((n_m, I // ML_QKV_BLOCK, ML_QKV_BLOCK, ML_QKV_BLOCK), ML_QKV_BLOCK ** -0.5),
        'ml_w_ig': nrm((n_m, 3 * I, 2 * NH), (3 * I) ** -0.5),
        'ml_b_ig': nrm((n_m, 2 * NH), 0.1),
        'ml_w_fg': nrm((n_m, 3 * I, 2 * NH), (3 * I) ** -0.5),
        'ml_b_fg': f_bias[None, :] + nrm((n_m, 2 * NH), 0.1),
        'ml_out_norm': 1.0 + nrm((n_m, I), 0.02),
        'ml_skip': 1.0 + nrm((n_m, I), 0.02),
        'ml_w_down': nrm((n_m, I, D), I ** -0.5),
        'mlp_w1': nrm((DEPTH, D, MLP_HIDDEN), D ** -0.5),
        'mlp_w2': nrm((DEPTH, MLP_HIDDEN, D), MLP_HIDDEN ** -0.5),
    }


def reference(x, norm_mix, norm_mlp, na_w_qkv, na_q_gain, na_k_gain, na_rel_bias, na_w_o,
              ml_w_up, ml_conv_w, ml_conv_b, ml_w_q, ml_w_k, ml_w_v, ml_w_ig, ml_b_ig,
              ml_w_fg, ml_b_fg, ml_out_norm, ml_skip, ml_w_down, mlp_w1, mlp_w2):
    for layer in range(DEPTH):
        j = layer // N_MIXERS
        hn = rms_norm(x, norm_mix[layer])
        if layer % N_MIXERS == 0:
            x = x + neighbourhood_attention(hn, na_w_qkv[j], na_q_gain[j], na_k_gain[j],
                                            na_rel_bias[j], na_w_o[j])
        else:
            x = x + mlstm_layer(hn, ml_w_up[j], ml_conv_w[j], ml_conv_b[j], ml_w_q[j], ml_w_k[j],
                                ml_w_v[j], ml_w_ig[j], ml_b_ig[j], ml_w_fg[j], ml_b_fg[j],
                                ml_out_norm[j], ml_skip[j], ml_w_down[j])
        x = x + sq_relu_mlp(rms_norm(x, norm_mlp[layer]), mlp_w1[layer], mlp_w2[layer])
    return x
```

```python
import numpy as np
from contextlib import ExitStack
import concourse.bass as bass
import concourse.mybir as mybir
from concourse.bass_utils import run_bass_kernel_spmd

F32 = mybir.dt.float32
BF16 = mybir.dt.bfloat16
AF = mybir.ActivationFunctionType
ALU = mybir.AluOpType

D = 2048
KC = D // 128
NCORE = 2
NSEG = 1
TOK = 8192 // NSEG
HALO = 256
TOKH = TOK + 2 * HALO
NBLK = TOK // 128
NSLOT = NBLK + 4
NHEAD = 16
HID = 8192
NEG = -30000.0


class Prog:
    def __init__(self, nc, stack, n_dma_sems=8):
        self.nc = nc
        self.eng = {"pe": nc.tensor, "act": nc.scalar, "dve": nc.vector,
                    "pool": nc.gpsimd, "sp": nc.sync}
        self.sem = {}
        self.cnt = {}
        for e in ["pe", "act", "dve", "pool"]:
            self.sem[e] = stack.enter_context(nc.semaphore("s_" + e))
            self.cnt[e] = 0
        self.dpool = {}
        for q in ["sp", "act", "pool"]:
            sems = [stack.enter_context(nc.semaphore("d_%s_%d" % (q, i))) for i in range(n_dma_sems)]
            self.dpool[q] = {"sems": sems, "vals": [0] * n_dma_sems, "next": 0}
        self.waited = {e: {} for e in self.eng}
        self.last_w = {}
        self.readers = {}
        self.n_ops = 0
        self.n_waits = 0

    def _wait(self, e, tok):
        sem, val, src = tok
        if src == "pe" and e == "pe":
            return
        sid = id(sem)
        if self.waited[e].get(sid, 0) >= val:
            return
        self.eng[e].wait_ge(sem, val)
        self.waited[e][sid] = val
        self.n_waits += 1

    def _deps(self, e, reads, writes):
        for k in reads:
            t = self.last_w.get(k)
            if t is not None:
                self._wait(e, t)
        for k in writes:
            t = self.last_w.get(k)
            if t is not None:
                self._wait(e, t)
            for t in self.readers.get(k, {}).values():
                self._wait(e, t)

    def _commit(self, tok, reads, writes):
        sid = id(tok[0])
        for k in reads:
            d = self.readers.setdefault(k, {})
            o = d.get(sid)
            if o is None or o[1] < tok[1]:
                d[sid] = tok
        for k in writes:
            self.last_w[k] = tok
            self.readers[k] = {}

    def op(self, e, fn, reads=(), writes=()):
        self._deps(e, reads, writes)
        ins = fn()
        self.cnt[e] += 1
        ins.then_inc(self.sem[e], 1)
        tok = (self.sem[e], self.cnt[e], e)
        self._commit(tok, reads, writes)
        self.n_ops += 1
        return tok

    def dma(self, q, out, in_, reads=(), writes=(), **kw):
        pool = self.dpool[q]
        i = pool["next"]
        pool["next"] = (i + 1) % len(pool["sems"])
        sem = pool["sems"][i]
        if pool["vals"][i] > 0:
            self._wait(q, (sem, pool["vals"][i], "dma"))
        self._deps(q, reads, writes)
        ins = self.eng[q].dma_start(out=out, in_=in_, **kw)
        pool["vals"][i] += 16
        ins.then_inc(sem, 16)
        tok = (sem, pool["vals"][i], "dma")
        self._commit(tok, reads, writes)
        self.n_ops += 1
        return tok

    def all_tokens(self):
        toks = [(self.sem[e], self.cnt[e], e) for e in self.cnt if self.cnt[e] > 0]
        for q, pool in self.dpool.items():
            for s, v in zip(pool["sems"], pool["vals"]):
                if v > 0:
                    toks.append((s, v, "dma"))
        return toks

    def barrier(self):
        toks = self.all_tokens()
        for e in self.eng:
            for t in toks:
                if t[2] == "pe" and e == "pe":
                    sid = id(t[0])
                    if self.waited[e].get(sid, 0) < t[1]:
                        self.eng[e].wait_ge(t[0], t[1])
                        self.waited[e][sid] = t[1]
                    continue
                self._wait(e, t)
        self.last_w = {}
        self.readers = {}

    def finish(self, e="sp"):
        for t in self.all_tokens():
            self._wait(e, t)


_UID = [0]


def mk_sbt(nc):
    _UID[0] += 1
    u = _UID[0]
    return lambda name, shape, dt: nc.sbuf_tensor("%s_u%d" % (name, u), shape, dt)


class Ctx:
    pass


def psbank(C, i):
    return C.psum[i // 2][:, (i % 2) * 512:(i % 2) * 512 + 512]


class WLoader:
    def __init__(self, P, nc, stg, stg_name):
        self.P, self.nc, self.stg, self.name = P, nc, stg, stg_name
        self.i = 0

    def load(self, wv, kc_n, col0, ncols, dst, dst_key):
        P, nc = self.P, self.nc
        per = max(1, 2048 // ncols)
        for p0 in range(0, kc_n, per):
            n = min(per, kc_n - p0)
            si = self.i % len(self.stg)
            self.i += 1
            s = self.stg[si]
            sv = s[:, 0:n * ncols].rearrange("p (c n) -> p c n", c=n)
            P.dma("sp", sv, wv[:, p0:p0 + n, col0:col0 + ncols], writes=[(self.name, si)])
            P.op("act", lambda sv=sv, p0=p0, n=n: nc.scalar.copy(out=dst[:, p0:p0 + n, :], in_=sv),
                 reads=[(self.name, si)], writes=[(dst_key, p0 // per)])
        return [(dst_key, i) for i in range((kc_n + per - 1) // per)]


def emit_rmsnorm_T(P, nc, C, X, xkey, hnT, hkey, gain, T, sq, sqkey, rtmp, rkey, psi):
    ps = psbank(C, psi)
    for th in range(T // 512):
        ts = slice(th * 512, (th + 1) * 512)
        for c in range(KC):
            s = sq[c % len(sq)]
            sk = (sqkey, c % len(sq))
            P.op("act", lambda s=s, c=c: nc.scalar.activation(out=s[:], in_=X[:, c, ts], func=AF.Square),
                 reads=[(xkey, c)], writes=[sk])
            P.op("pe", lambda s=s, c=c: nc.tensor.matmul(ps, lhsT=C.ones_f[:], rhs=s[:], start=(c == 0), stop=(c == KC - 1)),
                 reads=[sk], writes=[("ps", psi)])
        P.op("act", lambda: nc.scalar.activation(out=rtmp[:], in_=ps, func=AF.Sqrt, scale=1.0 / D, bias=C.eps6[:]),
             reads=[("ps", psi)], writes=[rkey])
        P.op("dve", lambda: nc.vector.reciprocal(out=rtmp[:], in_=rtmp[:]), reads=[rkey], writes=[rkey])
        for c in range(KC):
            P.op("dve", lambda c=c: nc.vector.scalar_tensor_tensor(
                out=hnT[:, c, ts], in0=X[:, c, ts], scalar=gain[:, c:c + 1], in1=rtmp[:],
                op0=ALU.mult, op1=ALU.mult),
                reads=[(xkey, c), rkey], writes=[(hkey, c)])


def emit_mlp_tile(P, nc, C, X, hnT, W1b, W2b, H, rl, wl, w1, w2, T, psA, psB):
    G = 512
    NG = HID // G
    NTH = T // 512
    w1v = w1.rearrange("(c p) n -> p c n", p=128)
    w2v = w2.rearrange("(c p) n -> p c n", p=128)
    st = {"a": 0, "b": 0}

    def load_w1(g):
        wl.load(w1v, KC, g * G, G, W1b[g % 2], ("W1b", g % 2))

    def load_w2(g):
        wl.load(w2v[:, g * 4:(g + 1) * 4, :], 4, 0, D, W2b[g % 2], ("W2b", g % 2))

    def stage_a(g):
        wb = g % 2
        for hc in range(4):
            for th in range(NTH):
                bi = psA[st["a"] % len(psA)]
                ri = st["a"] % len(rl)
                st["a"] += 1
                ps = psbank(C, bi)
                ts = slice(th * 512, (th + 1) * 512)

                def mm(ps=ps, hc=hc, ts=ts, wb=wb):
                    for kc in range(KC):
                        ins = nc.tensor.matmul(ps, lhsT=W1b[wb][:, kc, hc * 128:(hc + 1) * 128],
                                               rhs=hnT[:, kc, ts], start=(kc == 0), stop=(kc == KC - 1))
                    return ins
                P.op("pe", mm, reads=[(("W1b", wb), p) for p in range(KC // 4)] + [("hnT", c) for c in range(KC)],
                     writes=[("ps", bi)])
                r = rl[ri]
                P.op("act", lambda r=r, ps=ps: nc.scalar.activation(out=r[:], in_=ps, func=AF.Relu),
                     reads=[("ps", bi)], writes=[("rl", ri)])
                P.op("act", lambda r=r, hc=hc, ts=ts, wb=wb: nc.scalar.activation(
                    out=H[wb][:, hc, ts], in_=r[:], func=AF.Square),
                    reads=[("rl", ri)], writes=[("H", wb, hc, th)])

    def stage_b(g):
        wb = g % 2
        for dc in range(KC):
            for th in range(NTH):
                bi = psB[st["b"] % len(psB)]
                st["b"] += 1
                ps = psbank(C, bi)
                ts = slice(th * 512, (th + 1) * 512)

                def mm(ps=ps, dc=dc, ts=ts, wb=wb):
                    for hc in range(4):
                        ins = nc.tensor.matmul(ps, lhsT=W2b[wb][:, hc, dc * 128:(dc + 1) * 128],
                                               rhs=H[wb][:, hc, ts], start=(hc == 0), stop=(hc == 3))
                    return ins
                P.op("pe", mm, reads=[(("W2b", wb), hc) for hc in range(4)] + [("H", wb, hc, th) for hc in range(4)],
                     writes=[("ps", bi)])
                P.op("dve", lambda ps=ps, dc=dc, ts=ts: nc.vector.tensor_tensor(
                    out=X[:, dc, ts], in0=ps, in1=X[:, dc, ts], op=ALU.add),
                    reads=[("ps", bi), ("X", dc)], writes=[("X", dc)])

    load_w1(0)
    load_w2(0)
    for g in range(NG):
        if g + 1 < NG:
            load_w1(g + 1)
        stage_a(g)
        if g > 0:
            stage_b(g - 1)
        if g + 1 < NG:
            load_w2(g + 1)
    stage_b(NG - 1)


def emit_proj_accum_tile(P, nc, C, X, AT, atkey, Wb, wl, w, kc_n, T, psB):
    wv = w.rearrange("(c p) n -> p c n", p=128)
    NTH = T // 512
    NG = D // 512
    st = 0
    keys = {}
    keys[0] = wl.load(wv, kc_n, 0, 512, Wb[0], ("W1b", 0))
    for g in range(NG):
        if g + 1 < NG:
            keys[g + 1] = wl.load(wv, kc_n, (g + 1) * 512, 512, Wb[(g + 1) % 2], ("W1b", (g + 1) % 2))
        wb = g % 2
        for oc in range(4):
            dc = g * 4 + oc
            for th in range(NTH):
                bi = psB[st % len(psB)]
                st += 1
                ps = psbank(C, bi)
                ts = slice(th * 512, (th + 1) * 512)

                def mm(ps=ps, oc=oc, ts=ts, wb=wb):
                    for kc in range(kc_n):
                        ins = nc.tensor.matmul(ps, lhsT=Wb[wb][:, kc, oc * 128:(oc + 1) * 128],
                                               rhs=AT[:, kc, ts], start=(kc == 0), stop=(kc == kc_n - 1))
                    return ins
                P.op("pe", mm, reads=keys[g] + [(atkey, c) for c in range(kc_n)], writes=[("ps", bi)])
                P.op("dve", lambda ps=ps, dc=dc, ts=ts: nc.vector.tensor_tensor(
                    out=X[:, dc, ts], in0=ps, in1=X[:, dc, ts], op=ALU.add),
                    reads=[("ps", bi), ("X", dc)], writes=[("X", dc)])


def qknorm(P, nc, C, psi, gain_ap, eps_ap, scl, out_ap, okey, sqb, sqkey, rt, rtkey, pni):
    ps = psbank(C, psi)
    pn = psbank(C, pni)
    P.op("act", lambda: nc.scalar.activation(out=sqb[:], in_=ps, func=AF.Square),
         reads=[("ps", psi)], writes=[sqkey])
    P.op("pe", lambda: nc.tensor.matmul(pn, lhsT=C.ones_b[:], rhs=sqb[:], start=True, stop=True),
         reads=[sqkey], writes=[("ps", pni)])
    P.op("act", lambda: nc.scalar.activation(out=rt[:], in_=pn, func=AF.Sqrt, scale=scl, bias=eps_ap),
         reads=[("ps", pni)], writes=[rtkey])
    P.op("dve", lambda: nc.vector.reciprocal(out=rt[:], in_=rt[:]), reads=[rtkey], writes=[rtkey])
    P.op("dve", lambda: nc.vector.scalar_tensor_tensor(out=out_ap, in0=ps, scalar=gain_ap, in1=rt[:],
                                                       op0=ALU.mult, op1=ALU.mult),
         reads=[("ps", psi), rtkey], writes=[okey])


def stage_na1(P, nc, C, xhT, gains, wqkv, QT, KT, V):
    sbt = mk_sbt(nc)
    with ExitStack() as st:
        X = st.enter_context(sbt("n1X", [128, KC, 1024], F32))
        hnT = st.enter_context(sbt("n1h", [128, KC, 1024], BF16))
        Wb = [st.enter_context(sbt("n1W%d" % i, [128, KC, 512], BF16)) for i in range(2)]
        stg = [st.enter_context(sbt("n1s%d" % i, [128, 2048], F32)) for i in range(4)]
        sq = [st.enter_context(sbt("n1q%d" % i, [128, 512], F32)) for i in range(2)]
        rtmp = st.enter_context(sbt("n1r", [128, 512], F32))
        qsq = [st.enter_context(sbt("n1qs%d" % i, [128, 512], BF16)) for i in range(2)]
        qrt = [st.enter_context(sbt("n1qr%d" % i, [128, 512], F32)) for i in range(2)]
        qo = [st.enter_context(sbt("n1qo%d" % i, [128, 512], BF16)) for i in range(4)]
        wl = WLoader(P, nc, stg, "n1s")
        xv = xhT.rearrange("(c p) t -> p c t", p=128)
        wv = wqkv.rearrange("(c p) n -> p c n", p=128)
        Vv = V.rearrange("(c p) n -> p c n", p=128)
        tiles = [(i * 1024, 1024) for i in range(TOKH // 1024)] + [((TOKH // 1024) * 1024, 512)]
        seq = [(ti, g) for ti in range(len(tiles)) for g in range(12)]
        cnt = {"ps": 0, "q": 0, "o": 0}
        wkeys = {}
        wkeys[0] = wl.load(wv, KC, 0, 512, Wb[0], ("n1W", 0))
        for si, (ti, g) in enumerate(seq):
            t0, T = tiles[ti]
            if g == 0:
                for c in range(KC):
                    P.dma("sp", X[:, c, 0:T], xv[:, c, t0:t0 + T], writes=[("X", c)])
                emit_rmsnorm_T(P, nc, C, X, "X", hnT, "hnT", gains[:, 0:KC], T, sq, "n1q", rtmp, ("n1r",), 6)
            if si + 1 < len(seq):
                g2 = seq[si + 1][1]
                wkeys[si + 1] = wl.load(wv, KC, g2 * 512, 512, Wb[(si + 1) % 2], ("n1W", (si + 1) % 2))
            wb = si % 2
            hreads = [("hnT", c) for c in range(KC)]
            if g < 8:
                isq = g < 4
                dst = QT if isq else KT
                for hh in range(4):
                    head = (g % 4) * 4 + hh
                    for th in range(T // 512):
                        bi = cnt["ps"] % 4
                        cnt["ps"] += 1
                        ps = psbank(C, bi)
                        ts = slice(th * 512, (th + 1) * 512)

                        def mm(ps=ps, hh=hh, ts=ts, wb=wb):
                            for kc in range(KC):
                                ins = nc.tensor.matmul(ps, lhsT=Wb[wb][:, kc, hh * 128:(hh + 1) * 128],
                                                       rhs=hnT[:, kc, ts], start=(kc == 0), stop=(kc == KC - 1))
                            return ins
                        P.op("pe", mm, reads=wkeys[si] + hreads, writes=[("ps", bi)])
                        qi = cnt["q"] % 2
                        cnt["q"] += 1
                        oi = cnt["o"] % 4
                        cnt["o"] += 1
                        if isq:
                            qknorm(P, nc, C, bi, gains[:, 32:33], C.eps_q[:], 1.0, qo[oi][:], ("n1qo", oi),
                                   qsq[qi], ("n1qs", qi), qrt[qi], ("n1qr", qi), 4 + qi)
                        else:
                            qknorm(P, nc, C, bi, gains[:, 33:34], C.eps6[:], 1.0 / 128, qo[oi][:], ("n1qo", oi),
                                   qsq[qi], ("n1qs", qi), qrt[qi], ("n1qr", qi), 4 + qi)
                        P.dma("sp", dst[head, :, t0 + th * 512:t0 + (th + 1) * 512], qo[oi][:],
                              reads=[("n1qo", oi)], writes=[("QK", isq, head, t0 + th * 512)])
            else:
                for tb in range(T // 128):
                    bi = cnt["ps"] % 4
                    cnt["ps"] += 1
                    ps = psbank(C, bi)

                    def mm(ps=ps, tb=tb, wb=wb):
                        for kc in range(KC):
                            ins = nc.tensor.matmul(ps, lhsT=hnT[:, kc, tb * 128:(tb + 1) * 128],
                                                   rhs=Wb[wb][:, kc, :], start=(kc == 0), stop=(kc == KC - 1))
                        return ins
                    P.op("pe", mm, reads=wkeys[si] + hreads, writes=[("ps", bi)])
                    oi = cnt["o"] % 4
                    cnt["o"] += 1
                    P.op("act", lambda ps=ps, oi=oi: nc.scalar.copy(out=qo[oi][:], in_=ps),
                         reads=[("ps", bi)], writes=[("n1qo", oi)])
                    P.dma("sp", Vv[:, t0 // 128 + tb, (g - 8) * 512:(g - 7) * 512], qo[oi][:],
                          reads=[("n1qo", oi)], writes=[("Vd", g - 8, t0 // 128 + tb)])
        P.barrier()


def stage_na2(P, nc, C, QT, KT, V, btab, ATs):
    sbt = mk_sbt(nc)
    with ExitStack() as st:
        Kb = [st.enter_context(sbt("n2K%d" % i, [128, TOKH], BF16)) for i in range(2)]
        Qb = [st.enter_context(sbt("n2Q%d" % i, [128, TOKH], BF16)) for i in range(2)]
        Vb = [st.enter_context(sbt("n2V%d" % i, [128, NSLOT, 128], BF16)) for i in range(2)]
        Bs = [st.enter_context(sbt("n2Bs%d" % i, [128, 3200], F32)) for i in range(2)]
        Bb = [st.enter_context(sbt("n2Bb%d" % i, [128, 5, 640], BF16)) for i in range(2)]
        PT = [st.enter_context(sbt("n2P%d" % i, [128, 640], BF16)) for i in range(3)]
        Ah = [st.enter_context(sbt("n2A%d" % i, [128, TOK], BF16)) for i in range(2)]
        rd = [st.enter_context(sbt("n2r%d" % i, [128, 128], F32)) for i in range(2)]
        Vv = V.rearrange("(c p) n -> p c n", p=128)
        cnt = 0

        def load_head(h):
            hb = h % 2
            for s0 in range(0, NSLOT, 17):
                s1 = min(NSLOT, s0 + 17)
                P.dma("sp", Vb[hb][:, s0:s1, :], Vv[:, s0:s1, h * 128:(h + 1) * 128], writes=[("n2V", hb, s0)])
            P.dma("sp", Kb[hb][:], KT[h], writes=[("n2K", hb)])
            P.dma("sp", Qb[hb][:], QT[h], writes=[("n2Q", hb)])
            P.dma("sp", Bs[hb][:], btab[h], writes=[("n2Bs", hb)])
            P.op("act", lambda hb=hb: nc.scalar.copy(out=Bb[hb][:].rearrange("p a b -> p (a b)"), in_=Bs[hb][:]),
                 reads=[("n2Bs", hb)], writes=[("n2Bb", hb)])

        load_head(0)
        for h in range(NHEAD):
            if h + 1 < NHEAD:
                load_head(h + 1)
            hb = h % 2
            vb = hb
            hv = 0
            for j in range(NBLK):
                ty = {0: 0, 1: 1, NBLK - 2: 3, NBLK - 1: 4}.get(j, 2)
                si = cnt % 2
                pi = cnt % 3
                cnt += 1
                S = C.psum[si][:, 0:640]
                skeys = [("ps", 2 * si), ("ps", 2 * si + 1)]

                def mmS(S=S, j=j, hb=hb, ty=ty):
                    for c in range(5):
                        nc.tensor.matmul(S[:, c * 128:(c + 1) * 128], lhsT=Kb[hb][:, (j + c) * 128:(j + c + 1) * 128],
                                         rhs=Qb[hb][:, (j + 2) * 128:(j + 3) * 128], start=True, stop=False)
                        ins = nc.tensor.matmul(S[:, c * 128:(c + 1) * 128], lhsT=C.ident_b[:],
                                               rhs=Bb[hb][:, ty, c * 128:(c + 1) * 128], start=False, stop=True)
                    return ins
                P.op("pe", mmS, reads=[("n2K", hb), ("n2Q", hb), ("n2Bb", hb)], writes=skeys)
                P.op("act", lambda S=S, pi=pi: nc.scalar.activation(out=PT[pi][:], in_=S, func=AF.Exp),
                     reads=skeys, writes=[("n2P", pi)])
                oi = 4 + si
                O = psbank(C, oi)

                def mmO(O=O, j=j, vb=vb, hv=hv, pi=pi):
                    for c in range(5):
                        nc.tensor.matmul(O[:, 0:128], lhsT=Vb[vb][:, j + c, hv * 128:(hv + 1) * 128],
                                         rhs=PT[pi][:, c * 128:(c + 1) * 128], start=(c == 0), stop=(c == 4))
                    for c in range(5):
                        ins = nc.tensor.matmul(O[:, 128:256], lhsT=C.ones_b[:],
                                               rhs=PT[pi][:, c * 128:(c + 1) * 128], start=(c == 0), stop=(c == 4))
                    return ins
                P.op("pe", mmO, reads=[("n2V", vb, s0) for s0 in range(0, NSLOT, 17)] + [("n2P", pi)], writes=[("ps", oi)])
                P.op("dve", lambda O=O, si=si: nc.vector.reciprocal(out=rd[si][:], in_=O[:, 128:256]),
                     reads=[("ps", oi)], writes=[("n2r", si)])
                P.op("dve", lambda O=O, si=si, j=j, hb=hb: nc.vector.tensor_tensor(
                    out=Ah[hb][:, j * 128:(j + 1) * 128], in0=O[:, 0:128], in1=rd[si][:], op=ALU.mult),
                    reads=[("ps", oi), ("n2r", si)], writes=[("n2A", hb)])
            P.dma("sp", ATs[h * 128:(h + 1) * 128, :], Ah[hb][:], reads=[("n2A", hb)], writes=[("ATs", h)])
        P.barrier()


def stage_out_mlp(P, nc, C, x_src, x_col0, projs, gain_mlp, w1, w2, outT, out_col0=0):
    sbt = mk_sbt(nc)
    T = 1024
    with ExitStack() as st:
        X = st.enter_context(sbt("mX", [128, KC, T], F32))
        hnT = st.enter_context(sbt("mh", [128, KC, T], BF16))
        W1b = [st.enter_context(sbt("mW1%d" % i, [128, KC, 512], BF16)) for i in range(2)]
        W2b = [st.enter_context(sbt("mW2%d" % i, [128, 4, D], BF16)) for i in range(2)]
        stg = [st.enter_context(sbt("ms%d" % i, [128, 2048], F32)) for i in range(2)]
        H = [st.enter_context(sbt("mH%d" % i, [128, 4, T], BF16)) for i in range(2)]
        rl = [st.enter_context(sbt("mr%d" % i, [128, 512], F32)) for i in range(2)]
        rtmp = st.enter_context(sbt("mrt", [128, 512], F32))
        wl = WLoader(P, nc, stg, "ms")
        xv = x_src.rearrange("(c p) t -> p c t", p=128)
        ov = outT.rearrange("(c p) t -> p c t", p=128)
        for ti in range(TOK // T):
            for c in range(KC):
                P.dma("sp", X[:, c, :], xv[:, c, x_col0 + ti * T:x_col0 + (ti + 1) * T], writes=[("X", c)])
            for (AT, w) in projs:
                av = AT.rearrange("(c p) t -> p c t", p=128)
                for c in range(KC):
                    P.dma("sp", hnT[:, c, :], av[:, c, ti * T:(ti + 1) * T], writes=[("hnT", c)])
                emit_proj_accum_tile(P, nc, C, X, hnT, "hnT", W1b, wl, w, KC, T, [4, 5, 6, 7])
            emit_rmsnorm_T(P, nc, C, X, "X", hnT, "hnT", gain_mlp, T, rl, "rl", rtmp, ("mrt",), 3)
            emit_mlp_tile(P, nc, C, X, hnT, W1b, W2b, H, rl, wl, w1, w2, T, [0, 1, 2], [4, 5, 6, 7])
            for c in range(KC):
                P.dma("sp", ov[:, c, out_col0 + ti * T:out_col0 + (ti + 1) * T], X[:, c, :], reads=[("X", c)], writes=[("out", c, ti)])
        P.barrier()


def setup_ctx(P, nc, st, cst):
    C = Ctx()
    sbt = mk_sbt(nc)
    C.psum = [st.enter_context(nc.psum_tensor("ps%d" % i, [128, 1024], F32)) for i in range(4)]
    C.ones_f = st.enter_context(sbt("ones_f", [128, 128], F32))
    C.ones_b = st.enter_context(sbt("ones_b", [128, 128], BF16))
    C.ident_f = st.enter_context(sbt("ident_f", [128, 128], F32))
    C.ident_b = st.enter_context(sbt("ident_b", [128, 128], BF16))
    C.eps6 = st.enter_context(sbt("eps6", [128, 1], F32))
    C.eps_q = st.enter_context(sbt("eps_q", [128, 1], F32))
    C.eps5 = st.enter_context(sbt("eps5", [128, 1], F32))
    P.op("pool", lambda: nc.gpsimd.memset(C.ones_f[:], 1.0))
    P.op("pool", lambda: nc.gpsimd.memset(C.ones_b[:], 1.0))
    P.op("pool", lambda: nc.gpsimd.memset(C.eps6[:], 1e-6))
    P.op("pool", lambda: nc.gpsimd.memset(C.eps_q[:], 128e-6))
    P.op("pool", lambda: nc.gpsimd.memset(C.eps5[:], 1e-5))
    P.dma("sp", C.ident_f[:], cst[:, 0:128], writes=["identf"])
    P.op("pool", lambda: nc.gpsimd.tensor_copy(out=C.ident_b[:], in_=C.ident_f[:]), reads=["identf"], writes=["identb"])
    return C


def build_phase_a():
    nc = bass.Bass("TRN2", target_bir_lowering=False)
    dt = nc.dram_tensor
    xhT = dt("xhT", [D, TOKH], F32, kind="ExternalInput").ap()
    gains = dt("gains", [128, 34], F32, kind="ExternalInput").ap()
    cst = dt("cst", [128, 384], F32, kind="ExternalInput").ap()
    wqkv = dt("wqkv", [D, 3 * D], F32, kind="ExternalInput").ap()
    wo = dt("wo", [D, D], F32, kind="ExternalInput").ap()
    w1 = dt("w1", [D, HID], F32, kind="ExternalInput").ap()
    w2 = dt("w2", [HID, D], F32, kind="ExternalInput").ap()
    btab = dt("btab", [NHEAD, 128, 3200], F32, kind="ExternalInput").ap()
    outT = dt("outT", [D, TOK], F32, kind="ExternalOutput").ap()
    QT = dt("QTs", [NHEAD, 128, TOKH], BF16, kind="Internal").ap()
    KT = dt("KTs", [NHEAD, 128, TOKH], BF16, kind="Internal").ap()
    V = dt("Vs", [TOKH, D], BF16, kind="Internal").ap()
    ATs = dt("ATs", [D, TOK], BF16, kind="Internal").ap()
    with ExitStack() as st:
        P = Prog(nc, st)
        C = setup_ctx(P, nc, st, cst)
        gsb = st.enter_context(nc.sbuf_tensor("gains_sb", [128, 34], F32))
        P.dma("sp", gsb[:], gains)
        P.barrier()
        stage_na1(P, nc, C, xhT, gsb, wqkv, QT, KT, V)
        stage_na2(P, nc, C, QT, KT, V, btab, ATs)
        stage_out_mlp(P, nc, C, xhT, HALO, [(ATs, wo)], gsb[:, 16:32], w1, w2, outT)
        P.finish()
        print("phase A: ops", P.n_ops, "waits", P.n_waits)
    return nc


def make_cst():
    c = np.zeros((128, 384), np.float32)
    c[:, 0:128] = np.eye(128, dtype=np.float32)
    c[:, 128:256] = np.triu(np.ones((128, 128), np.float32))
    c[:, 256:384] = np.tril(np.ones((128, 128), np.float32))
    return c


def pgain(g):
    return np.ascontiguousarray(g.reshape(KC, 128).T)


def slot_chunks(seg):
    R0c = NBLK * seg
    sl = []
    for s in range(NSLOT):
        g = R0c - 2 + s
        sl.append(g if 0 <= g < 64 else None)
    if seg == 0:
        sl[0] = 3
    if seg == NSEG - 1:
        sl[NSLOT - 1] = 60
    return sl


def na_halo_xT(xb, seg):
    sl = slot_chunks(seg)
    xh = np.zeros((TOKH, D), np.float32)
    for s, g in enumerate(sl):
        if g is not None:
            xh[s * 128:(s + 1) * 128] = xb[g * 128:(g + 1) * 128]
    return np.ascontiguousarray(xh.T)


def na_bias_tables(rel_bias, seg):
    sl = slot_chunks(seg)
    out = np.full((NHEAD, 128, 5, 5, 128), NEG, np.float32)
    kk = np.arange(128)
    qq = np.arange(128)
    for ty, j in enumerate([0, 1, 5, NBLK - 2, NBLK - 1]):
        gq = NBLK * seg + j
        r = 2 * gq + qq // 64
        xq = qq % 64
        r0 = np.clip(r - 4, 0, 120)
        c0 = np.clip(xq - 8, 0, 48)
        for c in range(5):
            gk = sl[j + c]
            if gk is None:
                continue
            rk = (2 * gk + kk // 64)[:, None]
            xk = (kk % 64)[:, None]
            valid = (rk >= r0[None, :]) & (rk < r0[None, :] + 8) & (xk >= c0[None, :]) & (xk < c0[None, :] + 16)
            ir = np.clip(rk - r[None, :] + 7, 0, 14)
            ic = np.clip(xk - xq[None, :] + 15, 0, 30)
            vals = rel_bias[:, ir, ic]
            out[:, :, ty, c, :] = np.where(valid[None], vals, np.float32(NEG))
    return np.ascontiguousarray(out.reshape(NHEAD, 128, 3200))


INNER = 4096
ICH = INNER // 128
TB1 = 1024
TB1H = TB1 + 3


def stage_b1(P, nc, C, xcT, gain, w_up, cwt, cbt, bd, wg, gb, qT, kT, vT, xcoT, zsT, gates, dbg=9):
    sbt = mk_sbt(nc)
    with ExitStack() as st:
        hnT = st.enter_context(sbt("b1h", [128, KC, TB1H], BF16))
        xk = [st.enter_context(sbt("b1x%d" % i, [128, TB1H], F32)) for i in range(2)]
        sq = [st.enter_context(sbt("b1q%d" % i, [128, TB1H], F32)) for i in range(2)]
        rt = st.enter_context(sbt("b1rt", [128, TB1H], F32))
        Wb = [st.enter_context(sbt("b1W%d" % i, [128, KC, 512], BF16)) for i in range(2)]
        stg = [st.enter_context(sbt("b1s%d" % i, [128, 2048], F32)) for i in range(4)]
        xm = [st.enter_context(sbt("b1xm%d" % i, [128, TB1H], F32)) for i in range(2)]
        acc = [st.enter_context(sbt("b1ac%d" % i, [128, TB1], F32)) for i in range(2)]
        xc = [st.enter_context(sbt("b1xc%d" % i, [128, TB1], BF16)) for i in range(2)]
        xmb = [st.enter_context(sbt("b1xb%d" % i, [128, TB1], BF16)) for i in range(2)]
        qo = [st.enter_context(sbt("b1qo%d" % i, [128, TB1], BF16)) for i in range(4)]
        zo = [st.enter_context(sbt("b1zo%d" % i, [128, TB1], BF16)) for i in range(2)]
        bds = [st.enter_context(sbt("b1bs%d" % i, [128, 3, 128], F32)) for i in range(2)]
        bdb = [st.enter_context(sbt("b1bb%d" % i, [128, 3, 128], BF16)) for i in range(2)]
        wgs = st.enter_context(sbt("b1wgs", [128, 96 * 16], F32))
        wgb = st.enter_context(sbt("b1wgb", [128, 96, 16], BF16))
        cw = st.enter_context(sbt("b1cw", [128, ICH, 4], F32))
        cb = st.enter_context(sbt("b1cb", [128, ICH], F32))
        gbs = st.enter_context(sbt("b1gb", [16, 1], F32))
        go = st.enter_context(sbt("b1go", [16, TB1], F32))
        wl = WLoader(P, nc, stg, "b1s")
        P.dma("sp", wgs[:], wg.rearrange("p a b -> p (a b)"), writes=["wgs"])
        P.op("pool", lambda: nc.gpsimd.tensor_copy(out=wgb[:].rearrange("p a b -> p (a b)"), in_=wgs[:]), reads=["wgs"], writes=["wgb"])
        P.dma("sp", cw[:], cwt, writes=["cw"])
        P.dma("sp", cb[:], cbt, writes=["cb"])
        P.dma("sp", gbs[:], gb, writes=["gb"])
        xv = xcT.rearrange("(c p) t -> p c t", p=128)
        wv = w_up.rearrange("(c p) n -> p c n", p=128)
        subs = [(0, 512), (512, 512), (1024, 3)]
        cnt = {"ps": 0, "hw": 0, "q": 0, "x": 0, "z": 0}
        for ti in range(TOK // TB1):
            c0 = ti * TB1
            for c in range(KC):
                xi = cnt["x"] % 2
                cnt["x"] += 1
                P.dma("sp", xk[xi][:], xv[:, c, c0:c0 + TB1H], writes=[("b1x", xi)])
                P.op("act", lambda xi=xi: nc.scalar.activation(out=sq[xi][:], in_=xk[xi][:], func=AF.Square),
                     reads=[("b1x", xi)], writes=[("b1q", xi)])
                for si, (s0, sn) in enumerate(subs):
                    ps = psbank(C, 4 + si)[:, 0:sn]
                    P.op("pe", lambda ps=ps, xi=xi, s0=s0, sn=sn, c=c: nc.tensor.matmul(
                        ps, lhsT=C.ones_f[:], rhs=sq[xi][:, s0:s0 + sn], start=(c == 0), stop=(c == KC - 1)),
                        reads=[("b1q", xi)], writes=[("ps", 4 + si)])
            for si, (s0, sn) in enumerate(subs):
                ps = psbank(C, 4 + si)[:, 0:sn]
                P.op("act", lambda ps=ps, s0=s0, sn=sn: nc.scalar.activation(
                    out=rt[:, s0:s0 + sn], in_=ps, func=AF.Sqrt, scale=1.0 / D, bias=C.eps6[:]),
                    reads=[("ps", 4 + si)], writes=[("b1rt", si)])
            P.op("dve", lambda: nc.vector.reciprocal(out=rt[:], in_=rt[:]),
                 reads=[("b1rt", i) for i in range(3)], writes=[("b1rt", i) for i in range(3)])
            for c in range(KC):
                xi = cnt["x"] % 2
                cnt["x"] += 1
                P.dma("sp", xk[xi][:], xv[:, c, c0:c0 + TB1H], writes=[("b1x", xi)])
                P.op("dve", lambda xi=xi, c=c: nc.vector.scalar_tensor_tensor(
                    out=hnT[:, c, :], in0=xk[xi][:], scalar=gain[:, c:c + 1], in1=rt[:], op0=ALU.mult, op1=ALU.mult),
                    reads=[("b1x", xi)] + [("b1rt", i) for i in range(3)], writes=[("hnT", c)])
            hreads = [("hnT", c) for c in range(KC)]
            if dbg < 2:
                continue
            wk_ = wl.load(wv, KC, 0, 512, Wb[0], ("b1W", 0))
            for g in range(16):
                wkeys = wk_
                if g + 1 < 16:
                    wk_ = wl.load(wv, KC, (g + 1) * 512, 512, Wb[(g + 1) % 2], ("b1W", (g + 1) % 2))
                wb = g % 2
                for i in range(4):
                    if g < 8:
                        cc = g * 4 + i
                        mi = cnt["hw"] % 2
                        P.dma("sp", bds[mi][:], bd[cc], writes=[("b1bs", mi)])
                        P.op("act", lambda mi=mi: nc.scalar.copy(out=bdb[mi][:], in_=bds[mi][:]),
                             reads=[("b1bs", mi)], writes=[("b1bb", mi)])
                        for si, (s0, sn) in enumerate(subs):
                            bi = cnt["ps"] % 4
                            cnt["ps"] += 1
                            ps = psbank(C, bi)[:, 0:sn]

                            def mm(ps=ps, i=i, s0=s0, sn=sn, wb=wb):
                                for kc in range(KC):
                                    ins = nc.tensor.matmul(ps, lhsT=Wb[wb][:, kc, i * 128:(i + 1) * 128],
                                                           rhs=hnT[:, kc, s0:s0 + sn], start=(kc == 0), stop=(kc == KC - 1))
                                return ins
                            P.op("pe", mm, reads=wkeys + hreads, writes=[("ps", bi)])
                            P.op("act", lambda ps=ps, mi=mi, s0=s0, sn=sn: nc.scalar.copy(out=xm[mi][:, s0:s0 + sn], in_=ps),
                                 reads=[("ps", bi)], writes=[("b1xm", mi, si)])
                        xmk = [("b1xm", mi, si) for si in range(3)]
                        if dbg < 3:
                            cnt["hw"] += 1
                            continue
                        P.op("dve", lambda mi=mi, cc=cc: nc.vector.tensor_scalar(
                            out=acc[mi][:], in0=xm[mi][:, 0:TB1], scalar1=cw[:, cc, 0:1], scalar2=None, op0=ALU.mult),
                            reads=xmk + ["cw"], writes=[("b1ac", mi)])
                        for j in range(1, 4):
                            P.op("dve", lambda mi=mi, cc=cc, j=j: nc.vector.scalar_tensor_tensor(
                                out=acc[mi][:], in0=xm[mi][:, j:j + TB1], scalar=cw[:, cc, j:j + 1], in1=acc[mi][:],
                                op0=ALU.mult, op1=ALU.add),
                                reads=xmk + [("b1ac", mi)], writes=[("b1ac", mi)])
                        P.op("act", lambda mi=mi, cc=cc: nc.scalar.activation(
                            out=xc[mi][:], in_=acc[mi][:], func=AF.Silu, bias=cb[:, cc:cc + 1]),
                            reads=[("b1ac", mi), "cb"], writes=[("b1xc", mi)])
                        P.op("act", lambda mi=mi: nc.scalar.copy(out=xmb[mi][:], in_=xm[mi][:, 1:1 + TB1]),
                             reads=xmk, writes=[("b1xb", mi)])
                        P.dma("sp", xcoT[cc * 128:(cc + 1) * 128, c0:c0 + TB1], xc[mi][:], reads=[("b1xc", mi)],
                              writes=[("o_xc", cc, ti)])
                        cnt["hw"] += 1
                        if dbg < 4:
                            continue
                        for m, (src, skey, dst) in enumerate([(xc, "b1xc", qT), (xc, "b1xc", kT), (xmb, "b1xb", vT)]):
                            qi = cnt["q"] % 4
                            cnt["q"] += 1
                            for th in range(2):
                                bi = 6 + th
                                hb = cnt["ps"] % 4
                                cnt["ps"] += 1
                                ps = psbank(C, hb)
                                P.op("pe", lambda ps=ps, mi=mi, m=m, src=src, th=th: nc.tensor.matmul(
                                    ps, lhsT=bdb[mi][:, m, :], rhs=src[mi][:, th * 512:(th + 1) * 512], start=True, stop=True),
                                    reads=[("b1bb", mi), (skey, mi)], writes=[("ps", hb)])
                                if m >= 1:
                                    P.op("dve", lambda ps=ps, qi=qi, th=th: nc.vector.tensor_copy(
                                        out=qo[qi][:, th * 512:(th + 1) * 512], in_=ps),
                                        reads=[("ps", hb)], writes=[("b1qo", qi, th)])
                                else:
                                    P.op("act", lambda ps=ps, qi=qi, th=th: nc.scalar.copy(
                                        out=qo[qi][:, th * 512:(th + 1) * 512], in_=ps),
                                        reads=[("ps", hb)], writes=[("b1qo", qi, th)])
                                first = (cc == 0 and m == 0)
                                last = (cc == ICH - 1 and m == 2)
                                gps = psbank(C, bi)[0:16, :]
                                P.op("pe", lambda gps=gps, qi=qi, th=th, m=m, cc=cc, first=first, last=last: nc.tensor.matmul(
                                    gps, lhsT=wgb[:, m * 32 + cc, :], rhs=qo[qi][:, th * 512:(th + 1) * 512],
                                    start=first, stop=last),
                                    reads=[("b1qo", qi, th), "wgb"], writes=[("ps", bi)])
                            P.dma("sp", dst[cc * 128:(cc + 1) * 128, c0:c0 + TB1], qo[qi][:],
                                  reads=[("b1qo", qi, 0), ("b1qo", qi, 1)], writes=[("o_q", m, cc, ti)])
                    else:
                        if dbg < 5:
                            continue
                        cc = (g - 8) * 4 + i
                        zi = cnt["z"] % 2
                        cnt["z"] += 1
                        for th in range(2):
                            bi = cnt["ps"] % 4
                            cnt["ps"] += 1
                            ps = psbank(C, bi)

                            def mm(ps=ps, i=i, th=th, wb=wb):
                                for kc in range(KC):
                                    ins = nc.tensor.matmul(ps, lhsT=Wb[wb][:, kc, i * 128:(i + 1) * 128],
                                                           rhs=hnT[:, kc, 1 + th * 512:1 + (th + 1) * 512],
                                                           start=(kc == 0), stop=(kc == KC - 1))
                                return ins
                            P.op("pe", mm, reads=wkeys + hreads, writes=[("ps", bi)])
                            P.op("act", lambda ps=ps, zi=zi, th=th: nc.scalar.activation(
                                out=zo[zi][:, th * 512:(th + 1) * 512], in_=ps, func=AF.Silu),
                                reads=[("ps", bi)], writes=[("b1zo", zi, th)])
                        P.dma("sp", zsT[cc * 128:(cc + 1) * 128, c0:c0 + TB1], zo[zi][:],
                              reads=[("b1zo", zi, 0), ("b1zo", zi, 1)], writes=[("o_z", cc, ti)])
                if g == 7 and dbg >= 4:
                    for th in range(2):
                        gps = psbank(C, 6 + th)[0:16, :]
                        P.op("act", lambda gps=gps, th=th: nc.scalar.activation(
                            out=go[:, th * 512:(th + 1) * 512], in_=gps, func=AF.Identity, bias=gbs[:]),
                            reads=[("ps", 6 + th), "gb"], writes=[("b1go", th)])
                    P.dma("sp", gates[:, c0:c0 + TB1], go[:], reads=[("b1go", 0), ("b1go", 1)], writes=[("o_g", ti)])
        P.barrier()


def build_phase_b1(dbg=9):
    nc = bass.Bass("TRN2", target_bir_lowering=False)
    dt = nc.dram_tensor
    xcT = dt("xcT", [D, TOK + 3], F32, kind="ExternalInput").ap()
    gains = dt("gains", [128, KC], F32, kind="ExternalInput").ap()
    cst = dt("cst", [128, 384], F32, kind="ExternalInput").ap()
    w_up = dt("w_up", [D, 2 * INNER], F32, kind="ExternalInput").ap()
    cwt = dt("cwt", [128, ICH, 4], F32, kind="ExternalInput").ap()
    cbt = dt("cbt", [128, ICH], F32, kind="ExternalInput").ap()
    bd = dt("bd", [ICH, 128, 3, 128], F32, kind="ExternalInput").ap()
    wg = dt("wg", [128, 96, 16], F32, kind="ExternalInput").ap()
    gb = dt("gb", [16, 1], F32, kind="ExternalInput").ap()
    outs = {}
    for n in ["qT", "kT", "vT", "xcoT", "zsT"]:
        outs[n] = dt(n, [INNER, TOK], BF16, kind="ExternalOutput").ap()
    gates = dt("gates", [16, TOK], F32, kind="ExternalOutput").ap()
    with ExitStack() as st:
        P = Prog(nc, st)
        C = setup_ctx(P, nc, st, cst)
        gsb = st.enter_context(nc.sbuf_tensor("gains_sb", [128, KC], F32))
        P.dma("sp", gsb[:], gains)
        P.barrier()
        stage_b1(P, nc, C, xcT, gsb, w_up, cwt, cbt, bd, wg, gb, outs["qT"], outs["kT"], outs["vT"], outs["xcoT"], outs["zsT"], gates, dbg=dbg)
        P.finish()
        print("phase B1: ops", P.n_ops, "waits", P.n_waits)
    return nc


def b1_host_params(conv_w, conv_b, w_q, w_k, w_v, w_ig, b_ig, w_fg, b_fg):
    cwt = np.ascontiguousarray(conv_w.T.reshape(ICH, 128, 4).transpose(1, 0, 2))
    cbt = np.ascontiguousarray(conv_b.reshape(ICH, 128).T)
    bd = np.zeros((ICH, 128, 3, 128), np.float32)
    for m, w in enumerate([w_q, w_k, w_v]):
        wr = w.reshape(ICH, 32, 4, 4)
        for g in range(32):
            bd[:, 4 * g:4 * g + 4, m, 4 * g:4 * g + 4] = wr[:, g]
    W = np.concatenate([w_ig, w_fg], axis=1)
    wg = np.ascontiguousarray(W.reshape(3, ICH, 128, 16).transpose(2, 0, 1, 3).reshape(128, 96, 16))
    gb = np.concatenate([b_ig, b_fg]).reshape(16, 1).astype(np.float32)
    return dict(cwt=cwt, cbt=cbt, bd=bd, wg=wg, gb=gb)


SEQ = 8192
NCH = SEQ // 128
DH = 1024
DHC = DH // 128


def stage_b2_gates(P, nc, C, gin, grows, gsc, G):
    sbt = mk_sbt(nc)
    with ExitStack() as st:
        row = lambda n: st.enter_context(sbt(n, [1, SEQ], F32))
        zr = row("g_zr")
        ir = row("g_i")
        fr = row("g_f")
        t1 = row("g_t1")
        t2 = row("g_t2")
        mn = st.enter_context(sbt("g_mn", [1, NCH], F32))
        mp = st.enter_context(sbt("g_mp", [1, NCH], F32))
        one1 = C.ones_f[0:1, 0:1]
        P.op("pool", lambda: nc.gpsimd.memset(zr[:], 0.0), writes=["g_zr"])
        for dr in range(2):
            rev = (lambda t: t[:, ::-1]) if dr == 1 else (lambda t: t[:])
            ri, rf = grows[dr]
            P.dma("sp", t1[:], gin[ri:ri + 1, :], writes=["g_t1"])
            P.dma("sp", t2[:], gin[rf:rf + 1, :], writes=["g_t2"])
            P.op("dve", lambda: nc.vector.tensor_copy(out=ir[:], in_=rev(t1)), reads=["g_t1"], writes=["g_i"])
            P.op("dve", lambda: nc.vector.tensor_copy(out=fr[:], in_=rev(t2)), reads=["g_t2"], writes=["g_f"])
            P.op("act", lambda: nc.scalar.activation(out=fr[:], in_=fr[:], func=AF.Exp, scale=-1.0), reads=["g_f"], writes=["g_f"])
            P.op("act", lambda: nc.scalar.activation(out=fr[:], in_=fr[:], func=AF.Ln, bias=one1), reads=["g_f"], writes=["g_f"])
            P.op("dve", lambda: nc.vector.tensor_tensor_scan(out=t1[:], data0=fr[:], data1=zr[:], initial=0.0,
                                                             op0=ALU.add, op1=ALU.add),
                 reads=["g_f", "g_zr"], writes=["g_t1"])
            P.op("dve", lambda: nc.vector.tensor_tensor(out=ir[:], in0=ir[:], in1=t1[:], op=ALU.add),
                 reads=["g_i", "g_t1"], writes=["g_i"])
            P.op("dve", lambda: nc.vector.tensor_tensor_scan(out=t2[:], data0=ir[:], data1=ir[:], initial=0.0,
                                                             op0=ALU.max, op1=ALU.max),
                 reads=["g_i"], writes=["g_t2"])
            P.op("dve", lambda: nc.vector.tensor_copy(out=mn[:], in_=t2[:, 127::128]), reads=["g_t2"], writes=["g_mn"])
            P.op("pool", lambda: nc.gpsimd.memset(mp[:], 0.0), writes=["g_mp"])
            P.op("dve", lambda: nc.vector.tensor_copy(out=mp[:, 1:NCH], in_=mn[:, 0:NCH - 1]), reads=["g_mn", "g_mp"], writes=["g_mp"])
            P.op("dve", lambda: nc.vector.tensor_copy(out=fr[:], in_=rev(ir)), reads=["g_i", "g_f"], writes=["g_f"])
            P.dma("sp", gsc[dr, 0:1, :], fr[:], reads=["g_f"], writes=[("gsc", dr, 0)])
            P.op("dve", lambda: nc.vector.tensor_copy(out=t2[:], in_=rev(t1)), reads=["g_t1", "g_t2"], writes=["g_t2"])
            P.dma("sp", gsc[dr, 1:2, :], t2[:], reads=["g_t2"], writes=[("gsc", dr, 1)])
            P.op("dve", lambda: nc.vector.tensor_copy(out=t1[:, 0:NCH], in_=rev(mn)), reads=["g_mn", "g_t1"], writes=["g_t1"])
            P.op("dve", lambda: nc.vector.tensor_copy(out=t1[:, NCH:2 * NCH], in_=rev(mp)), reads=["g_mp", "g_t1"], writes=["g_t1"])
            P.dma("sp", gsc[dr, 2:3, 0:2 * NCH], t1[:, 0:2 * NCH], reads=["g_t1"], writes=[("gsc", dr, 2)])
            g = G[dr]
            P.dma("sp", g["A"][:], gsc[dr, 0, :].rearrange("(c p) -> p c", p=128), reads=[("gsc", dr, 0)],
                  writes=[("gA", dr)], allow_slow_non_contiguous=True)
            P.dma("sp", g["F"][:], gsc[dr, 1, :].rearrange("(c p) -> p c", p=128), reads=[("gsc", dr, 1)],
                  writes=[("gF", dr)], allow_slow_non_contiguous=True)
            P.dma("sp", g["MN"][:], gsc[dr, 2:3, 0:NCH].broadcast_to([128, NCH]), reads=[("gsc", dr, 2)], writes=[("gMN", dr)])
            P.dma("sp", g["MP"][:], gsc[dr, 2:3, NCH:2 * NCH].broadcast_to([128, NCH]), reads=[("gsc", dr, 2)], writes=[("gMP", dr)])
            for name, x, y in [("U", "A", "MP"), ("E2", "F", "MP"), ("WK", "A", "MN"), ("DEC", "MP", "MN")]:
                P.op("dve", lambda name=name, x=x, y=y: nc.vector.tensor_tensor(out=g[name][:], in0=g[x][:], in1=g[y][:], op=ALU.subtract),
                     reads=[("g" + x, dr), ("g" + y, dr)], writes=[("g" + name, dr)])
                P.op("act", lambda name=name: nc.scalar.activation(out=g[name][:], in_=g[name][:], func=AF.Exp),
                     reads=[("g" + name, dr)], writes=[("g" + name, dr)])
                if name in ("U", "WK"):
                    P.op("dve", lambda name=name: nc.vector.tensor_scalar(out=g[name][:], in0=g[name][:], scalar1=DH ** -0.5,
                                                                          scalar2=None, op0=ALU.mult),
                         reads=[("g" + name, dr)], writes=[("g" + name, dr)])
        P.barrier()


def stage_b2(P, nc, C, qTh, kTh, kt, vt, xct, zst, gin, onorm, skipv, cst, gsc, HFs, yt, nsteps=NCH, grows=((0, 1), (2, 3))):
    sbt = mk_sbt(nc)
    with ExitStack() as st:
        G = [{n: st.enter_context(sbt("G%d%s" % (dr, n), [128, NCH], F32)) for n in ["A", "F", "MN", "MP", "U", "E2", "WK", "DEC"]}
             for dr in range(2)]
        stage_b2_gates(P, nc, C, gin, grows, gsc, G)
        QT2 = [st.enter_context(sbt("s_q%d" % i, [128, DHC, 512], BF16)) for i in range(2)]
        KT2 = [st.enter_context(sbt("s_k%d" % i, [128, DHC, 512], BF16)) for i in range(2)]
        KTk = [st.enter_context(sbt("s_kt%d" % i, [128, 4, DH], BF16)) for i in range(2)]
        VTk = [st.enter_context(sbt("s_vt%d" % i, [128, 4, DH], BF16)) for i in range(2)]
        Cst = st.enter_context(sbt("s_C", [128, DHC, DH], F32))
        Cb = st.enter_context(sbt("s_Cb", [128, DHC, DH], BF16))
        nst = st.enter_context(sbt("s_n", [128, DHC], F32))
        nb = st.enter_context(sbt("s_nb", [128, DHC], BF16))
        SD = [st.enter_context(sbt("s_sd%d" % i, [128, 128], BF16)) for i in range(2)]
        wk = [st.enter_context(sbt("s_wk%d" % i, [128, DH], BF16)) for i in range(2)]
        hout = [st.enter_context(sbt("s_h%d" % i, [128, DH], F32)) for i in range(2)]
        rr = [st.enter_context(sbt("s_r%d" % i, [128, 1], F32)) for i in range(2)]
        tri = [st.enter_context(sbt("s_tri%d" % i, [128, 128], F32)) for i in range(2)]
        HF = [st.enter_context(sbt("s_hf%d" % i, [128, DH], F32)) for i in range(2)]
        XC = [st.enter_context(sbt("s_xc%d" % i, [128, DH], BF16)) for i in range(2)]
        ZS = [st.enter_context(sbt("s_zs%d" % i, [128, DH], BF16)) for i in range(2)]
        T1 = [st.enter_context(sbt("s_t1%d" % i, [128, DH], F32)) for i in range(2)]
        T3 = st.enter_context(sbt("s_t3", [128, DH], F32))
        Y = [st.enter_context(sbt("s_y%d" % i, [128, DH], BF16)) for i in range(2)]
        bst = st.enter_context(sbt("s_bst", [128, 2, 6], F32))
        mv = st.enter_context(sbt("s_mv", [128, 2], F32))
        rs = st.enter_context(sbt("s_rs", [128, 1], F32))
        on_sb = st.enter_context(sbt("s_on", [128, DH], F32))
        sk_sb = st.enter_context(sbt("s_sk", [128, DH], F32))
        P.dma("sp", tri[0][:], cst[:, 128:256], writes=[("tri", 0)])
        P.dma("sp", tri[1][:], cst[:, 256:384], writes=[("tri", 1)])
        P.dma("sp", on_sb[:], onorm, writes=["on"])
        P.dma("sp", sk_sb[:], skipv, writes=["sk"])
        qv = qTh.rearrange("(c p) t -> p c t", p=128)
        kv = kTh.rearrange("(c p) t -> p c t", p=128)
        ktv = kt.rearrange("(c p) d -> p c d", p=128)
        vtv = vt.rearrange("(c p) d -> p c d", p=128)
        Ckeys = [("C", dc, eh) for dc in range(DHC) for eh in range(2)]
        Cbkeys = [("Cb", dc, eh) for dc in range(DHC) for eh in range(2)]
        cnt = {"dc": 0, "cast": 0}

        def load_super(sc, buf):
            P.dma("sp", QT2[buf][:], qv[:, :, sc * 512:(sc + 1) * 512], writes=[("s_q", buf)])
            P.dma("sp", KT2[buf][:], kv[:, :, sc * 512:(sc + 1) * 512], writes=[("s_k", buf)])
            P.dma("sp", KTk[buf][:], ktv[:, sc * 4:(sc + 1) * 4, :], writes=[("s_kt", buf)])
            P.dma("sp", VTk[buf][:], vtv[:, sc * 4:(sc + 1) * 4, :], writes=[("s_vt", buf)])

        for dr in (range(2) if nsteps == NCH else range(1)):
            g = G[dr]
            P.op("pool", lambda: nc.gpsimd.memset(Cst[:], 0.0), writes=Ckeys)
            P.op("pool", lambda: nc.gpsimd.memset(Cb[:], 0.0), writes=Cbkeys)
            P.op("pool", lambda: nc.gpsimd.memset(nst[:], 0.0), writes=["n"])
            P.op("pool", lambda: nc.gpsimd.memset(nb[:], 0.0), writes=["nb"])
            order = list(range(NCH)) if dr == 0 else list(range(NCH - 1, -1, -1))
            order = order[:nsteps]
            scs = []
            for c in order:
                if not scs or scs[-1] != c // 4:
                    scs.append(c // 4)
            sbuf_of = {sc: i % 2 for i, sc in enumerate(scs)}
            load_super(scs[0], 0)
            for k, c in enumerate(order):
                sc = c // 4
                lc = c % 4
                buf = sbuf_of[sc]
                first_in_sc = (k == 0) or (order[k - 1] // 4 != sc)
                if first_in_sc:
                    i = scs.index(sc)
                    if i + 1 < len(scs):
                        load_super(scs[i + 1], (i + 1) % 2)
                par = k % 2
                tsl = slice(lc * 128, (lc + 1) * 128)
                if dr == 1:
                    P.dma("sp", HF[par][:], HFs[c * 128:(c + 1) * 128, :], reads=[("HFs", c)], writes=[("s_hf", par)])
                    P.dma("sp", XC[par][:], xct[c * 128:(c + 1) * 128, :], writes=[("s_xc", par)])
                    P.dma("sp", ZS[par][:], zst[c * 128:(c + 1) * 128, :], writes=[("s_zs", par)])
                sA = psbank(C, par)[:, 0:128]

                def mmS(sA=sA, buf=buf, tsl=tsl):
                    for dc in range(DHC):
                        ins = nc.tensor.matmul(sA, lhsT=KT2[buf][:, dc, tsl], rhs=QT2[buf][:, dc, tsl],
                                               start=(dc == 0), stop=(dc == DHC - 1))
                    return ins
                P.op("pe", mmS, reads=[("s_k", buf), ("s_q", buf)], writes=[("ps", par)])
                P.op("dve", lambda sA=sA, par=par, c=c, dr=dr: nc.vector.scalar_tensor_tensor(
                    out=SD[par][:], in0=sA, scalar=g["U"][:, c:c + 1], in1=tri[dr][:], op0=ALU.mult, op1=ALU.mult),
                    reads=[("ps", par), ("gU", dr), ("tri", dr)], writes=[("s_sd", par)])
                for eh in range(2):
                    nb_ = 2 + eh
                    ps = psbank(C, nb_)

                    def mmN(ps=ps, eh=eh, par=par, buf=buf, lc=lc, tsl=tsl):
                        nc.tensor.matmul(ps, lhsT=SD[par][:], rhs=VTk[buf][:, lc, eh * 512:(eh + 1) * 512], start=True, stop=False)
                        for dc in range(DHC):
                            ins = nc.tensor.matmul(ps, lhsT=QT2[buf][:, dc, tsl], rhs=Cb[:, dc, eh * 512:(eh + 1) * 512],
                                                   start=False, stop=(dc == DHC - 1))
                        return ins
                    P.op("pe", mmN, reads=[("s_sd", par), ("s_vt", buf), ("s_q", buf)] + [("Cb", dc, eh) for dc in range(DHC)],
                         writes=[("ps", nb_)])
                dn = psbank(C, 4)[:, par:par + 1]

                def mmD(dn=dn, par=par, buf=buf, tsl=tsl):
                    nc.tensor.matmul(dn, lhsT=SD[par][:], rhs=C.ones_b[:, 0:1], start=True, stop=False)
                    for dc in range(DHC):
                        ins = nc.tensor.matmul(dn, lhsT=QT2[buf][:, dc, tsl], rhs=nb[:, dc:dc + 1], start=False, stop=(dc == DHC - 1))
                    return ins
                P.op("pe", mmD, reads=[("s_sd", par), ("s_q", buf), "nb"], writes=[("ps4", par)])
                P.op("act", lambda dn=dn, par=par: nc.scalar.activation(out=rr[par][:], in_=dn, func=AF.Abs),
                     reads=[("ps4", par)], writes=[("s_r", par)])
                P.op("dve", lambda par=par, c=c: nc.vector.tensor_scalar(
                    out=rr[par][:], in0=rr[par][:], scalar1=g["E2"][:, c:c + 1], scalar2=None, op0=ALU.max),
                    reads=[("s_r", par), ("gE2", dr)], writes=[("s_r", par)])
                P.op("dve", lambda par=par: nc.vector.reciprocal(out=rr[par][:], in_=rr[par][:]), reads=[("s_r", par)], writes=[("s_r", par)])
                for eh in range(2):
                    ps = psbank(C, 2 + eh)
                    P.op("act", lambda ps=ps, eh=eh, par=par: nc.scalar.activation(
                        out=hout[par][:, eh * 512:(eh + 1) * 512], in_=ps, func=AF.Copy, scale=rr[par][:, 0:1]),
                        reads=[("ps", 2 + eh), ("s_r", par)], writes=[("s_h", par, eh)])
                hk = [("s_h", par, 0), ("s_h", par, 1)]
                if dr == 0:
                    P.dma("sp", HFs[c * 128:(c + 1) * 128, :], hout[par][:], reads=hk, writes=[("HFs", c)])
                else:
                    t1 = T1[par]
                    P.op("dve", lambda par=par, t1=t1: nc.vector.tensor_tensor(out=t1[:], in0=hout[par][:], in1=HF[par][:], op=ALU.add),
                         reads=hk + [("s_hf", par)], writes=[("s_t1", par)])
                    for hh in range(2):
                        P.op("dve", lambda hh=hh, t1=t1: nc.vector.bn_stats(out=bst[:, hh, :], in_=t1[:, hh * 512:(hh + 1) * 512]),
                             reads=[("s_t1", par)], writes=[("bst", hh)])
                    P.op("dve", lambda: nc.vector.bn_aggr(out=mv[:], in_=bst[:].rearrange("p a b -> p (a b)")),
                         reads=[("bst", 0), ("bst", 1)], writes=["mv"])
                    P.op("act", lambda: nc.scalar.activation(out=rs[:], in_=mv[:, 1:2], func=AF.Sqrt, bias=C.eps5[:]),
                         reads=["mv"], writes=["rs"])
                    P.op("dve", lambda: nc.vector.reciprocal(out=rs[:], in_=rs[:]), reads=["rs"], writes=["rs"])
                    P.op("dve", lambda t1=t1: nc.vector.tensor_scalar(out=t1[:], in0=t1[:], scalar1=mv[:, 0:1], scalar2=rs[:, 0:1],
                                                                      op0=ALU.subtract, op1=ALU.mult),
                         reads=[("s_t1", par), "mv", "rs"], writes=[("s_t1", par)])
                    P.op("dve", lambda t1=t1: nc.vector.tensor_tensor(out=t1[:], in0=t1[:], in1=on_sb[:], op=ALU.mult),
                         reads=[("s_t1", par), "on"], writes=[("s_t1", par)])
                    P.op("pool", lambda par=par: nc.gpsimd.tensor_tensor(out=T3[:], in0=XC[par][:], in1=sk_sb[:], op=ALU.mult),
                         reads=[("s_xc", par), "sk"], writes=["s_t3"])
                    P.op("dve", lambda t1=t1: nc.vector.tensor_tensor(out=t1[:], in0=t1[:], in1=T3[:], op=ALU.add),
                         reads=[("s_t1", par), "s_t3"], writes=[("s_t1", par)])
                    P.op("pool", lambda par=par, t1=t1: nc.gpsimd.tensor_tensor(out=Y[par][:], in0=t1[:], in1=ZS[par][:], op=ALU.mult),
                         reads=[("s_t1", par), ("s_zs", par)], writes=[("s_y", par)])
                    P.dma("sp", yt[c * 128:(c + 1) * 128, :], Y[par][:], reads=[("s_y", par)], writes=[("yt", c)])
                P.op("act", lambda par=par, buf=buf, lc=lc, c=c: nc.scalar.activation(
                    out=wk[par][:], in_=KTk[buf][:, lc, :], func=AF.Copy, scale=g["WK"][:, c:c + 1]),
                    reads=[("s_kt", buf), ("gWK", dr)], writes=[("s_wk", par)])
                for dc in range(DHC):
                    for eh in range(2):
                        bi = 5 + cnt["dc"] % 3
                        cnt["dc"] += 1
                        ps = psbank(C, bi)
                        esl = slice(eh * 512, (eh + 1) * 512)
                        P.op("pe", lambda ps=ps, par=par, dc=dc, buf=buf, lc=lc, esl=esl: nc.tensor.matmul(
                            ps, lhsT=wk[par][:, dc * 128:(dc + 1) * 128], rhs=VTk[buf][:, lc, esl], start=True, stop=True),
                            reads=[("s_wk", par), ("s_vt", buf)], writes=[("ps", bi)])
                        P.op("dve", lambda ps=ps, dc=dc, esl=esl, c=c: nc.vector.scalar_tensor_tensor(
                            out=Cst[:, dc, esl], in0=Cst[:, dc, esl], scalar=g["DEC"][:, c:c + 1], in1=ps, op0=ALU.mult, op1=ALU.add),
                            reads=[("ps", bi), ("C", dc, eh), ("gDEC", dr)], writes=[("C", dc, eh)])
                        ce = "act"
                        cnt["cast"] += 1
                        if ce == "act":
                            P.op("act", lambda dc=dc, esl=esl: nc.scalar.copy(out=Cb[:, dc, esl], in_=Cst[:, dc, esl]),
                                 reads=[("C", dc, eh)], writes=[("Cb", dc, eh)])
                        else:
                            P.op("pool", lambda dc=dc, esl=esl: nc.gpsimd.tensor_copy(out=Cb[:, dc, esl], in_=Cst[:, dc, esl]),
                                 reads=[("C", dc, eh)], writes=[("Cb", dc, eh)])
                dnn = psbank(C, 4)[:, 8 + 8 * par:16 + 8 * par]

                def mmn(dnn=dnn, par=par):
                    for dc in range(DHC):
                        ins = nc.tensor.matmul(dnn[:, dc:dc + 1], lhsT=wk[par][:, dc * 128:(dc + 1) * 128], rhs=C.ones_b[:, 0:1],
                                               start=True, stop=True)
                    return ins
                P.op("pe", mmn, reads=[("s_wk", par)], writes=[("ps4n", par)])
                P.op("dve", lambda dnn=dnn, c=c: nc.vector.scalar_tensor_tensor(
                    out=nst[:], in0=nst[:], scalar=g["DEC"][:, c:c + 1], in1=dnn, op0=ALU.mult, op1=ALU.add),
                    reads=[("ps4n", par), "n", ("gDEC", dr)], writes=["n"])
                P.op("dve", lambda: nc.vector.tensor_copy(out=nb[:], in_=nst[:]), reads=["n"], writes=["nb"])
        P.barrier()


def build_phase_b2(nsteps=NCH):
    nc = bass.Bass("TRN2", target_bir_lowering=False)
    dt = nc.dram_tensor
    qTh = dt("qTh", [DH, SEQ], BF16, kind="ExternalInput").ap()
    kTh = dt("kTh", [DH, SEQ], BF16, kind="ExternalInput").ap()
    kt = dt("kt", [SEQ, DH], BF16, kind="ExternalInput").ap()
    vt = dt("vt", [SEQ, DH], BF16, kind="ExternalInput").ap()
    xct = dt("xct", [SEQ, DH], BF16, kind="ExternalInput").ap()
    zst = dt("zst", [SEQ, DH], BF16, kind="ExternalInput").ap()
    gin = dt("gin", [4, SEQ], F32, kind="ExternalInput").ap()
    onorm = dt("onorm", [128, DH], F32, kind="ExternalInput").ap()
    skipv = dt("skipv", [128, DH], F32, kind="ExternalInput").ap()
    cst = dt("cst", [128, 384], F32, kind="ExternalInput").ap()
    yt = dt("yt", [SEQ, DH], BF16, kind="ExternalOutput").ap()
    gsc = dt("gsc", [2, 3, SEQ], F32, kind="Internal").ap()
    HFs = dt("HFs", [SEQ, DH], F32, kind="Internal").ap()
    with ExitStack() as st:
        P = Prog(nc, st)
        C = setup_ctx(P, nc, st, cst)
        P.barrier()
        stage_b2(P, nc, C, qTh, kTh, kt, vt, xct, zst, gin, onorm, skipv, cst, gsc, HFs, yt, nsteps=nsteps)
        P.finish()
        print("phase B2: ops", P.n_ops, "waits", P.n_waits)
    return nc


def build_phase_b3():
    nc = bass.Bass("TRN2", target_bir_lowering=False)
    dt = nc.dram_tensor
    xT = dt("xT", [D, TOK], F32, kind="ExternalInput").ap()
    yT = dt("yT", [INNER, TOK], BF16, kind="ExternalInput").ap()
    gains = dt("gains", [128, KC], F32, kind="ExternalInput").ap()
    cst = dt("cst", [128, 384], F32, kind="ExternalInput").ap()
    wd = dt("wd", [INNER, D], F32, kind="ExternalInput").ap()
    w1 = dt("w1", [D, HID], F32, kind="ExternalInput").ap()
    w2 = dt("w2", [HID, D], F32, kind="ExternalInput").ap()
    outT = dt("outT", [D, TOK], F32, kind="ExternalOutput").ap()
    with ExitStack() as st:
        P = Prog(nc, st)
        C = setup_ctx(P, nc, st, cst)
        gsb = st.enter_context(nc.sbuf_tensor("gains_sb", [128, KC], F32))
        P.dma("sp", gsb[:], gains)
        P.barrier()
        stage_out_mlp(P, nc, C, xT, 0, [(yT[0:D, :], wd[0:D, :]), (yT[D:2 * D, :], wd[D:2 * D, :])], gsb, w1, w2, outT)
        P.finish()
        print("phase B3: ops", P.n_ops, "waits", P.n_waits)
    return nc


_PROGS = {}


def _prog(name):
    if name not in _PROGS:
        _PROGS[name] = {"A": build_phase_a, "B1": build_phase_b1, "B2": build_phase_b2, "B3": build_phase_b3}[name]()
    return _PROGS[name]


def _run(name, in_maps):
    res = run_bass_kernel_spmd(_prog(name), in_maps, core_ids=list(range(NCORE)))
    return res.results


def kernel_unfused_8core(x, norm_mix, norm_mlp, na_w_qkv, na_q_gain, na_k_gain, na_rel_bias, na_w_o,
           ml_w_up, ml_conv_w, ml_conv_b, ml_w_q, ml_w_k, ml_w_v, ml_w_ig, ml_b_ig,
           ml_w_fg, ml_b_fg, ml_out_norm, ml_skip, ml_w_down, mlp_w1, mlp_w2):
    f32 = lambda a: np.ascontiguousarray(np.asarray(a, dtype=np.float32))
    x = f32(x)
    cst = make_cst()
    B = 2
    xs = [x[b] for b in range(B)]
    xT_core = None
    for layer in range(4):
        j = layer // 2
        w1, w2 = f32(mlp_w1[layer]), f32(mlp_w2[layer])
        if layer % 2 == 0:
            gains = f32(np.concatenate([pgain(f32(norm_mix[layer])), pgain(f32(norm_mlp[layer])),
                                        f32(na_q_gain[j])[:, None], f32(na_k_gain[j])[:, None]], axis=1))
            wqkv, wo = f32(na_w_qkv[j]), f32(na_w_o[j])
            rb = f32(na_rel_bias[j])
            btabs = [na_bias_tables(rb, seg) for seg in range(4)]
            ims = []
            for core in range(NCORE):
                b, seg = core // 4, core % 4
                ims.append({"xhT": na_halo_xT(xs[b], seg), "gains": gains, "cst": cst, "wqkv": wqkv, "wo": wo,
                            "w1": w1, "w2": w2, "btab": btabs[seg]})
            res = _run("A", ims)
            xT_core = [r["outT"] for r in res]
        else:
            hp = b1_host_params(f32(ml_conv_w[j]), f32(ml_conv_b[j]), f32(ml_w_q[j]), f32(ml_w_k[j]), f32(ml_w_v[j]),
                                f32(ml_w_ig[j]), f32(ml_b_ig[j]), f32(ml_w_fg[j]), f32(ml_b_fg[j]))
            gmix = pgain(f32(norm_mix[layer]))
            wup = f32(ml_w_up[j])
            ims = []
            for core in range(NCORE):
                b, seg = core // 4, core % 4
                xc = np.zeros((TOK + 3, D), np.float32)
                lo, hi = seg * TOK - 1, seg * TOK + TOK + 2
                a, e = max(lo, 0), min(hi, SEQ)
                xc[a - lo:e - lo] = xs[b][a:e]
                ims.append({"xcT": np.ascontiguousarray(xc.T), "gains": gmix, "cst": cst, "w_up": wup, **hp})
            r1 = _run("B1", ims)
            on, sk = f32(ml_out_norm[j]), f32(ml_skip[j])
            ims = []
            for core in range(NCORE):
                b, h = core // 4, core % 4
                hs = slice(h * DH, (h + 1) * DH)
                cat = lambda n: np.concatenate([r1[b * 4 + s][n][hs, :] for s in range(4)], axis=1)
                qTh, kTh, vTh, xcTh, zsTh = cat("qT"), cat("kT"), cat("vT"), cat("xcoT"), cat("zsT")
                gfull = np.concatenate([r1[b * 4 + s]["gates"] for s in range(4)], axis=1)
                gin = np.ascontiguousarray(gfull[[h, 8 + h, 4 + h, 12 + h], :])
                ims.append({"qTh": np.ascontiguousarray(qTh), "kTh": np.ascontiguousarray(kTh),
                            "kt": np.ascontiguousarray(kTh.T), "vt": np.ascontiguousarray(vTh.T),
                            "xct": np.ascontiguousarray(xcTh.T), "zst": np.ascontiguousarray(zsTh.T),
                            "gin": gin, "onorm": np.ascontiguousarray(np.tile(on[hs], (128, 1))),
                            "skipv": np.ascontiguousarray(np.tile(sk[hs], (128, 1))), "cst": cst})
            r2 = _run("B2", ims)
            gmlp = pgain(f32(norm_mlp[layer]))
            wd = f32(ml_w_down[j])
            ims = []
            for core in range(NCORE):
                b, seg = core // 4, core % 4
                ts = slice(seg * TOK, (seg + 1) * TOK)
                yT = np.concatenate([r2[b * 4 + h]["yt"][ts, :].T for h in range(4)], axis=0)
                ims.append({"xT": xT_core[core], "yT": np.ascontiguousarray(yT), "gains": gmlp, "cst": cst,
                            "wd": wd, "w1": w1, "w2": w2})
            res = _run("B3", ims)
            xT_core = [r["outT"] for r in res]
        xs = [np.concatenate([xT_core[b * 4 + s].T for s in range(4)], axis=0) for b in range(B)]
    return np.ascontiguousarray(np.stack(xs, axis=0).astype(np.float32))


def stage_transpose(P, nc, C, src, dst, R, Cc):
    sbt = mk_sbt(nc)
    with ExitStack() as st:
        S = [st.enter_context(sbt("tS%d" % i, [128, 8, 512], BF16)) for i in range(2)]
        O = [st.enter_context(sbt("tO%d" % i, [128, 1024], BF16)) for i in range(4)]
        sv = src.rearrange("(c p) n -> p c n", p=128)
        it = 0
        oc = 0
        for rg in range(R // 1024):
            for cg in range(Cc // 512):
                sb = it % 2
                it += 1
                P.dma("sp", S[sb][:], sv[:, rg * 8:(rg + 1) * 8, cg * 512:(cg + 1) * 512], writes=[("tS", sb)])
                for q in range(4):
                    bi = oc % 4
                    oi = oc % 4
                    oc += 1
                    pv = psbank(C, bi).bitcast(BF16)

                    def tr(pv=pv, sb=sb, q=q):
                        for rb in range(8):
                            ins = nc.tensor.transpose(out=pv[:, rb * 128:(rb + 1) * 128], in_=S[sb][:, rb, q * 128:(q + 1) * 128],
                                                      identity=C.ident_b[:])
                        return ins
                    P.op("pe", tr, reads=[("tS", sb)], writes=[("ps", bi)])
                    if oc % 2 == 0:
                        P.op("act", lambda pv=pv, oi=oi: nc.scalar.copy(out=O[oi][:], in_=pv), reads=[("ps", bi)], writes=[("tO", oi)])
                    else:
                        P.op("dve", lambda pv=pv, oi=oi: nc.vector.tensor_copy(out=O[oi][:], in_=pv), reads=[("ps", bi)], writes=[("tO", oi)])
                    c0 = cg * 512 + q * 128
                    P.dma("sp", dst[c0:c0 + 128, rg * 1024:(rg + 1) * 1024], O[oi][:], reads=[("tO", oi)], writes=[("tD", c0, rg)])
        P.barrier()


def build_fused(layers=4):
    nc = bass.Bass("TRN2", target_bir_lowering=False)
    dt = nc.dram_tensor
    ein = lambda n, shp, t=F32: dt(n, shp, t, kind="ExternalInput").ap()
    scr = lambda n, shp, t: dt(n, shp, t, kind="Internal").ap()
    xhT = ein("xhT", [D, TOKH])
    cst = ein("cst", [128, 384])
    gna = ein("gna", [2, 128, 34])
    gml = ein("gml", [2, 128, 32])
    wqkv = ein("wqkv", [2, D, 3 * D])
    wo = ein("wo", [2, D, D])
    btab = ein("btab", [2, NHEAD, 128, 3200])
    w1 = ein("w1", [4, D, HID])
    w2 = ein("w2", [4, HID, D])
    w_up = ein("w_up", [2, D, 2 * INNER])
    cwt = ein("cwt", [2, 128, ICH, 4])
    cbt = ein("cbt", [2, 128, ICH])
    bd = ein("bd", [2, ICH, 128, 3, 128])
    wg = ein("wg", [2, 128, 96, 16])
    gb = ein("gb", [2, 16, 1])
    onb = ein("onb", [2, 4, 128, DH])
    skb = ein("skb", [2, 4, 128, DH])
    wd = ein("wd", [2, INNER, D])
    outT = dt("outT", [D, TOK], F32, kind="ExternalOutput").ap()
    XS = scr("XS", [D, TOKH], F32)
    QT = scr("QTs", [NHEAD, 128, TOKH], BF16)
    KT = scr("KTs", [NHEAD, 128, TOKH], BF16)
    V = scr("Vs", [TOKH, D], BF16)
    ATs = scr("ATs", [D, TOK], BF16)
    fm = {n: scr(n, [INNER, TOK], BF16) for n in ["qT", "kT", "vT", "xcoT", "zsT"]}
    tm = {n: scr(n, [TOK, INNER], BF16) for n in ["kt", "vt", "xct", "zst", "yt"]}
    yT = scr("yT", [INNER, TOK], BF16)
    gates = scr("gates", [16, TOK], F32)
    gsc = scr("gsc", [2, 3, SEQ], F32)
    HFs = scr("HFs", [SEQ, DH], F32)
    with ExitStack() as st:
        P = Prog(nc, st)
        C = setup_ctx(P, nc, st, cst)
        gna_sb = [st.enter_context(nc.sbuf_tensor("gna%d" % i, [128, 34], F32)) for i in range(2)]
        gml_sb = [st.enter_context(nc.sbuf_tensor("gml%d" % i, [128, 32], F32)) for i in range(2)]
        for i in range(2):
            P.dma("sp", gna_sb[i][:], gna[i])
            P.dma("sp", gml_sb[i][:], gml[i])
        for c in range(KC):
            P.dma("sp", XS[c * 128:(c + 1) * 128, :], xhT[c * 128:(c + 1) * 128, :])
        P.barrier()
        for layer in range(layers):
            j = layer // 2
            last = (layer == layers - 1)
            dst, dcol = (outT, 0) if last else (XS, HALO)
            if layer % 2 == 0:
                stage_na1(P, nc, C, XS, gna_sb[j], wqkv[j], QT, KT, V)
                stage_na2(P, nc, C, QT, KT, V, btab[j], ATs)
                stage_out_mlp(P, nc, C, XS, HALO, [(ATs, wo[j])], gna_sb[j][:, 16:32], w1[layer], w2[layer], dst, dcol)
            else:
                stage_b1(P, nc, C, XS[:, HALO - 1:HALO + TOK + 2], gml_sb[j][:, 0:16], w_up[j], cwt[j], cbt[j], bd[j], wg[j], gb[j],
                         fm["qT"], fm["kT"], fm["vT"], fm["xcoT"], fm["zsT"], gates)
                for a, b in [("kT", "kt"), ("vT", "vt"), ("xcoT", "xct"), ("zsT", "zst")]:
                    stage_transpose(P, nc, C, fm[a], tm[b], INNER, TOK)
                for h in range(4):
                    hs = slice(h * DH, (h + 1) * DH)
                    stage_b2(P, nc, C, fm["qT"][hs, :], fm["kT"][hs, :], tm["kt"][:, hs], tm["vt"][:, hs], tm["xct"][:, hs],
                             tm["zst"][:, hs], gates, onb[j, h], skb[j, h], cst, gsc, HFs, tm["yt"][:, hs],
                             grows=((h, 8 + h), (4 + h, 12 + h)))
                stage_transpose(P, nc, C, tm["yt"], yT, TOK, INNER)
                stage_out_mlp(P, nc, C, XS, HALO, [(yT[0:D, :], wd[j, 0:D, :]), (yT[D:2 * D, :], wd[j, D:2 * D, :])],
                              gml_sb[j][:, 16:32], w1[layer], w2[layer], dst, dcol)
                if not last:
                    for c in range(KC):
                        rs_ = slice(c * 128, (c + 1) * 128)
                        P.dma("sp", XS[rs_, 0:128], XS[rs_, HALO + 3 * 128:HALO + 4 * 128])
                        P.dma("sp", XS[rs_, TOKH - 128:TOKH], XS[rs_, HALO + 60 * 128:HALO + 61 * 128])
                    P.barrier()
        P.finish()
        print("fused: ops", P.n_ops, "waits", P.n_waits)
    return nc


def kernel(x, norm_mix, norm_mlp, na_w_qkv, na_q_gain, na_k_gain, na_rel_bias, na_w_o,
           ml_w_up, ml_conv_w, ml_conv_b, ml_w_q, ml_w_k, ml_w_v, ml_w_ig, ml_b_ig,
           ml_w_fg, ml_b_fg, ml_out_norm, ml_skip, ml_w_down, mlp_w1, mlp_w2):
    f32 = lambda a: np.ascontiguousarray(np.asarray(a, dtype=np.float32))
    x = f32(x)
    norm_mix, norm_mlp = f32(norm_mix), f32(norm_mlp)
    gna = np.stack([np.concatenate([pgain(norm_mix[2 * j]), pgain(norm_mlp[2 * j]),
                                    f32(na_q_gain[j])[:, None], f32(na_k_gain[j])[:, None]], axis=1) for j in range(2)])
    gml = np.stack([np.concatenate([pgain(norm_mix[2 * j + 1]), pgain(norm_mlp[2 * j + 1])], axis=1) for j in range(2)])
    hps = [b1_host_params(f32(ml_conv_w[j]), f32(ml_conv_b[j]), f32(ml_w_q[j]), f32(ml_w_k[j]), f32(ml_w_v[j]),
                          f32(ml_w_ig[j]), f32(ml_b_ig[j]), f32(ml_w_fg[j]), f32(ml_b_fg[j])) for j in range(2)]
    stk = lambda k: np.ascontiguousarray(np.stack([hps[j][k] for j in range(2)]))
    on, sk = f32(ml_out_norm), f32(ml_skip)
    rep = lambda v: np.ascontiguousarray(np.stack([np.stack([np.tile(v[j, h * DH:(h + 1) * DH], (128, 1)) for h in range(4)])
                                                   for j in range(2)]))
    common = {
        "cst": make_cst(), "gna": f32(gna), "gml": f32(gml), "wqkv": f32(na_w_qkv), "wo": f32(na_w_o),
        "btab": np.ascontiguousarray(np.stack([na_bias_tables(f32(na_rel_bias[j]), 0) for j in range(2)])),
        "w1": f32(mlp_w1), "w2": f32(mlp_w2), "w_up": f32(ml_w_up),
        "cwt": stk("cwt"), "cbt": stk("cbt"), "bd": stk("bd"), "wg": stk("wg"), "gb": stk("gb"),
        "onb": rep(on), "skb": rep(sk), "wd": f32(ml_w_down),
    }
    ims = [dict(common, xhT=na_halo_xT(x[b], 0)) for b in range(NCORE)]
    if "fused" not in _PROGS:
        _PROGS["fused"] = build_fused()
    res = run_bass_kernel_spmd(_PROGS["fused"], ims, core_ids=list(range(NCORE)))
    out = np.stack([res.results[b]["outT"].T for b in range(NCORE)], axis=0)
    return np.ascontiguousarray(out.astype(np.float32))
```

```python
import numpy as np
from contextlib import ExitStack
import concourse.bass as bass
import concourse.mybir as mybir
from concourse.bass_utils import run_bass_kernel_spmd

F32 = mybir.dt.float32
BF16 = mybir.dt.bfloat16
AF = mybir.ActivationFunctionType
ALU = mybir.AluOpType

D = 2048
KC = D // 128
NCORE = 2
NSEG = 1
TOK = 8192 // NSEG
HALO = 256
TOKH = TOK + 2 * HALO
NBLK = TOK // 128
NSLOT = NBLK + 4
NHEAD = 16
HID = 8192
NEG = -30000.0


class Prog:
    def __init__(self, nc, stack, n_dma_sems=8):
        self.nc = nc
        self.eng = {"pe": nc.tensor, "act": nc.scalar, "dve": nc.vector,
                    "pool": nc.gpsimd, "sp": nc.sync}
        self.sem = {}
        self.cnt = {}
        for e in ["pe", "act", "dve", "pool"]:
            self.sem[e] = stack.enter_context(nc.semaphore("s_" + e))
            self.cnt[e] = 0
        self.dpool = {}
        for q in ["sp", "act", "pool"]:
            sems = [stack.enter_context(nc.semaphore("d_%s_%d" % (q, i))) for i in range(n_dma_sems)]
            self.dpool[q] = {"sems": sems, "vals": [0] * n_dma_sems, "next": 0}
        self.waited = {e: {} for e in self.eng}
        self.last_w = {}
        self.readers = {}
        self.n_ops = 0
        self.n_waits = 0

    def _wait(self, e, tok):
        sem, val, src = tok
        if src == "pe" and e == "pe":
            return
        sid = id(sem)
        if self.waited[e].get(sid, 0) >= val:
            return
        self.eng[e].wait_ge(sem, val)
        self.waited[e][sid] = val
        self.n_waits += 1

    def _deps(self, e, reads, writes):
        for k in reads:
            t = self.last_w.get(k)
            if t is not None:
                self._wait(e, t)
        for k in writes:
            t = self.last_w.get(k)
            if t is not None:
                self._wait(e, t)
            for t in self.readers.get(k, {}).values():
                self._wait(e, t)

    def _commit(self, tok, reads, writes):
        sid = id(tok[0])
        for k in reads:
            d = self.readers.setdefault(k, {})
            o = d.get(sid)
            if o is None or o[1] < tok[1]:
                d[sid] = tok
        for k in writes:
            self.last_w[k] = tok
            self.readers[k] = {}

    def op(self, e, fn, reads=(), writes=()):
        self._deps(e, reads, writes)
        ins = fn()
        self.cnt[e] += 1
        ins.then_inc(self.sem[e], 1)
        tok = (self.sem[e], self.cnt[e], e)
        self._commit(tok, reads, writes)
        self.n_ops += 1
        return tok

    def dma(self, q, out, in_, reads=(), writes=(), **kw):
        pool = self.dpool[q]
        i = pool["next"]
        pool["next"] = (i + 1) % len(pool["sems"])
        sem = pool["sems"][i]
        if pool["vals"][i] > 0:
            self._wait(q, (sem, pool["vals"][i], "dma"))
        self._deps(q, reads, writes)
        ins = self.eng[q].dma_start(out=out, in_=in_, **kw)
        pool["vals"][i] += 16
        ins.then_inc(sem, 16)
        tok = (sem, pool["vals"][i], "dma")
        self._commit(tok, reads, writes)
        self.n_ops += 1
        return tok

    def all_tokens(self):
        toks = [(self.sem[e], self.cnt[e], e) for e in self.cnt if self.cnt[e] > 0]
        for q, pool in self.dpool.items():
            for s, v in zip(pool["sems"], pool["vals"]):
                if v > 0:
                    toks.append((s, v, "dma"))
        return toks

    def barrier(self):
        toks = self.all_tokens()
        for e in self.eng:
            for t in toks:
                if t[2] == "pe" and e == "pe":
                    sid = id(t[0])
                    if self.waited[e].get(sid, 0) < t[1]:
                        self.eng[e].wait_ge(t[0], t[1])
                        self.waited[e][sid] = t[1]
                    continue
                self._wait(e, t)
        self.last_w = {}
        self.readers = {}

    def finish(self, e="sp"):
        for t in self.all_tokens():
            self._wait(e, t)


_UID = [0]


def mk_sbt(nc):
    _UID[0] += 1
    u = _UID[0]
    return lambda name, shape, dt: nc.sbuf_tensor("%s_u%d" % (name, u), shape, dt)


class Ctx:
    pass


def psbank(C, i):
    return C.psum[i // 2][:, (i % 2) * 512:(i % 2) * 512 + 512]


class WLoader:
    def __init__(self, P, nc, stg, stg_name):
        self.P, self.nc, self.stg, self.name = P, nc, stg, stg_name
        self.i = 0

    def load(self, wv, kc_n, col0, ncols, dst, dst_key):
        P = self.P
        per = max(1, 2048 // ncols)
        for p0 in range(0, kc_n, per):
            n = min(per, kc_n - p0)
            P.dma("pool", dst[:, p0:p0 + n, :], wv[:, p0:p0 + n, col0:col0 + ncols], writes=[(dst_key, p0 // per)])
        return [(dst_key, i) for i in range((kc_n + per - 1) // per)]


def emit_rmsnorm_T(P, nc, C, X, xkey, hnT, hkey, gain, T, sq, sqkey, rtmp, rkey, psi):
    ps = psbank(C, psi)
    for th in range(T // 512):
        ts = slice(th * 512, (th + 1) * 512)
        for c in range(KC):
            s = sq[c % len(sq)]
            sk = (sqkey, c % len(sq))
            P.op("act", lambda s=s, c=c: nc.scalar.activation(out=s[:], in_=X[:, c, ts], func=AF.Square),
                 reads=[(xkey, c)], writes=[sk])
            P.op("pe", lambda s=s, c=c: nc.tensor.matmul(ps, lhsT=C.ones_f[:], rhs=s[:], start=(c == 0), stop=(c == KC - 1)),
                 reads=[sk], writes=[("ps", psi)])
        P.op("act", lambda: nc.scalar.activation(out=rtmp[:], in_=ps, func=AF.Sqrt, scale=1.0 / D, bias=C.eps6[:]),
             reads=[("ps", psi)], writes=[rkey])
        P.op("dve", lambda: nc.vector.reciprocal(out=rtmp[:], in_=rtmp[:]), reads=[rkey], writes=[rkey])
        for c in range(KC):
            P.op("dve", lambda c=c: nc.vector.scalar_tensor_tensor(
                out=hnT[:, c, ts], in0=X[:, c, ts], scalar=gain[:, c:c + 1], in1=rtmp[:],
                op0=ALU.mult, op1=ALU.mult),
                reads=[(xkey, c), rkey], writes=[(hkey, c)])


def emit_mlp_tile(P, nc, C, X, hnT, W1b, W2b, H, rl, wl, w1, w2, T, psA, psB):
    G = 512
    NG = HID // G
    NTH = T // 512
    w1v = w1.rearrange("(c p) n -> p c n", p=128)
    w2v = w2.rearrange("(c p) n -> p c n", p=128)
    st = {"a": 0, "b": 0}

    def load_w1(g):
        wl.load(w1v, KC, g * G, G, W1b[g % 2], ("W1b", g % 2))

    def load_w2(g):
        wl.load(w2v[:, g * 4:(g + 1) * 4, :], 4, 0, D, W2b[g % 2], ("W2b", g % 2))

    def stage_a(g):
        wb = g % 2
        for hc in range(4):
            for th in range(NTH):
                bi = psA[st["a"] % len(psA)]
                ri = st["a"] % len(rl)
                st["a"] += 1
                ps = psbank(C, bi)
                ts = slice(th * 512, (th + 1) * 512)

                def mm(ps=ps, hc=hc, ts=ts, wb=wb):
                    for kc in range(KC):
                        ins = nc.tensor.matmul(ps, lhsT=W1b[wb][:, kc, hc * 128:(hc + 1) * 128],
                                               rhs=hnT[:, kc, ts], start=(kc == 0), stop=(kc == KC - 1))
                    return ins
                P.op("pe", mm, reads=[(("W1b", wb), p) for p in range(KC // 4)] + [("hnT", c) for c in range(KC)],
                     writes=[("ps", bi)])
                r = rl[ri]
                P.op("act", lambda r=r, ps=ps: nc.scalar.activation(out=r[:], in_=ps, func=AF.Relu),
                     reads=[("ps", bi)], writes=[("rl", ri)])
                P.op("act", lambda r=r, hc=hc, ts=ts, wb=wb: nc.scalar.activation(
                    out=H[wb][:, hc, ts], in_=r[:], func=AF.Square),
                    reads=[("rl", ri)], writes=[("H", wb, hc, th)])

    def stage_b(g):
        wb = g % 2
        for dc in range(KC):
            for th in range(NTH):
                bi = psB[st["b"] % len(psB)]
                st["b"] += 1
                ps = psbank(C, bi)
                ts = slice(th * 512, (th + 1) * 512)

                def mm(ps=ps, dc=dc, ts=ts, wb=wb):
                    for hc in range(4):
                        ins = nc.tensor.matmul(ps, lhsT=W2b[wb][:, hc, dc * 128:(dc + 1) * 128],
                                               rhs=H[wb][:, hc, ts], start=(hc == 0), stop=(hc == 3))
                    return ins
                P.op("pe", mm, reads=[(("W2b", wb), hc) for hc in range(4)] + [("H", wb, hc, th) for hc in range(4)],
                     writes=[("ps", bi)])
                P.op("dve", lambda ps=ps, dc=dc, ts=ts: nc.vector.tensor_tensor(
                    out=X[:, dc, ts], in0=ps, in1=X[:, dc, ts], op=ALU.add),
                    reads=[("ps", bi), ("X", dc)], writes=[("X", dc)])

    load_w1(0)
    load_w2(0)
    for g in range(NG):
        if g + 1 < NG:
            load_w1(g + 1)
        stage_a(g)
        if g > 0:
            stage_b(g - 1)
        if g + 1 < NG:
            load_w2(g + 1)
    stage_b(NG - 1)


def emit_proj_accum_tile(P, nc, C, X, AT, atkey, Wb, wl, w, kc_n, T, psB):
    wv = w.rearrange("(c p) n -> p c n", p=128)
    NTH = T // 512
    NG = D // 512
    st = 0
    keys = {}
    keys[0] = wl.load(wv, kc_n, 0, 512, Wb[0], ("W1b", 0))
    for g in range(NG):
        if g + 1 < NG:
            keys[g + 1] = wl.load(wv, kc_n, (g + 1) * 512, 512, Wb[(g + 1) % 2], ("W1b", (g + 1) % 2))
        wb = g % 2
        for oc in range(4):
            dc = g * 4 + oc
            for th in range(NTH):
                bi = psB[st % len(psB)]
                st += 1
                ps = psbank(C, bi)
                ts = slice(th * 512, (th + 1) * 512)

                def mm(ps=ps, oc=oc, ts=ts, wb=wb):
                    for kc in range(kc_n):
                        ins = nc.tensor.matmul(ps, lhsT=Wb[wb][:, kc, oc * 128:(oc + 1) * 128],
                                               rhs=AT[:, kc, ts], start=(kc == 0), stop=(kc == kc_n - 1))
                    return ins
                P.op("pe", mm, reads=keys[g] + [(atkey, c) for c in range(kc_n)], writes=[("ps", bi)])
                P.op("dve", lambda ps=ps, dc=dc, ts=ts: nc.vector.tensor_tensor(
                    out=X[:, dc, ts], in0=ps, in1=X[:, dc, ts], op=ALU.add),
                    reads=[("ps", bi), ("X", dc)], writes=[("X", dc)])


def qknorm(P, nc, C, psi, gain_ap, eps_ap, scl, out_ap, okey, sqb, sqkey, rt, rtkey, pni):
    ps = psbank(C, psi)
    pn = psbank(C, pni)
    P.op("act", lambda: nc.scalar.activation(out=sqb[:], in_=ps, func=AF.Square),
         reads=[("ps", psi)], writes=[sqkey])
    P.op("pe", lambda: nc.tensor.matmul(pn, lhsT=C.ones_b[:], rhs=sqb[:], start=True, stop=True),
         reads=[sqkey], writes=[("ps", pni)])
    P.op("act", lambda: nc.scalar.activation(out=rt[:], in_=pn, func=AF.Sqrt, scale=scl, bias=eps_ap),
         reads=[("ps", pni)], writes=[rtkey])
    P.op("dve", lambda: nc.vector.reciprocal(out=rt[:], in_=rt[:]), reads=[rtkey], writes=[rtkey])
    P.op("dve", lambda: nc.vector.scalar_tensor_tensor(out=out_ap, in0=ps, scalar=gain_ap, in1=rt[:],
                                                       op0=ALU.mult, op1=ALU.mult),
         reads=[("ps", psi), rtkey], writes=[okey])


def stage_na1(P, nc, C, xhT, gains, wqkv, QT, KT, V):
    sbt = mk_sbt(nc)
    with ExitStack() as st:
        X = st.enter_context(sbt("n1X", [128, KC, 1024], F32))
        hnT = st.enter_context(sbt("n1h", [128, KC, 1024], BF16))
        Wb = [st.enter_context(sbt("n1W%d" % i, [128, KC, 512], BF16)) for i in range(2)]
        stg = [st.enter_context(sbt("n1s%d" % i, [128, 2048], F32)) for i in range(4)]
        sq = [st.enter_context(sbt("n1q%d" % i, [128, 512], F32)) for i in range(2)]
        rtmp = st.enter_context(sbt("n1r", [128, 512], F32))
        qsq = [st.enter_context(sbt("n1qs%d" % i, [128, 512], BF16)) for i in range(2)]
        qrt = [st.enter_context(sbt("n1qr%d" % i, [128, 512], F32)) for i in range(2)]
        qo = [st.enter_context(sbt("n1qo%d" % i, [128, 512], BF16)) for i in range(4)]
        wl = WLoader(P, nc, stg, "n1s")
        xv = xhT.rearrange("(c p) t -> p c t", p=128)
        wv = wqkv.rearrange("(c p) n -> p c n", p=128)
        Vv = V.rearrange("(c p) n -> p c n", p=128)
        tiles = [(i * 1024, 1024) for i in range(TOKH // 1024)] + [((TOKH // 1024) * 1024, 512)]
        seq = [(ti, g) for ti in range(len(tiles)) for g in range(12)]
        cnt = {"ps": 0, "q": 0, "o": 0}
        wkeys = {}
        wkeys[0] = wl.load(wv, KC, 0, 512, Wb[0], ("n1W", 0))
        for si, (ti, g) in enumerate(seq):
            t0, T = tiles[ti]
            if g == 0:
                for c in range(KC):
                    P.dma("sp", X[:, c, 0:T], xv[:, c, t0:t0 + T], writes=[("X", c)])
                emit_rmsnorm_T(P, nc, C, X, "X", hnT, "hnT", gains[:, 0:KC], T, sq, "n1q", rtmp, ("n1r",), 6)
            if si + 1 < len(seq):
                g2 = seq[si + 1][1]
                wkeys[si + 1] = wl.load(wv, KC, g2 * 512, 512, Wb[(si + 1) % 2], ("n1W", (si + 1) % 2))
            wb = si % 2
            hreads = [("hnT", c) for c in range(KC)]
            if g < 8:
                isq = g < 4
                dst = QT if isq else KT
                for hh in range(4):
                    head = (g % 4) * 4 + hh
                    for th in range(T // 512):
                        bi = cnt["ps"] % 4
                        cnt["ps"] += 1
                        ps = psbank(C, bi)
                        ts = slice(th * 512, (th + 1) * 512)

                        def mm(ps=ps, hh=hh, ts=ts, wb=wb):
                            for kc in range(KC):
                                ins = nc.tensor.matmul(ps, lhsT=Wb[wb][:, kc, hh * 128:(hh + 1) * 128],
                                                       rhs=hnT[:, kc, ts], start=(kc == 0), stop=(kc == KC - 1))
                            return ins
                        P.op("pe", mm, reads=wkeys[si] + hreads, writes=[("ps", bi)])
                        qi = cnt["q"] % 2
                        cnt["q"] += 1
                        oi = cnt["o"] % 4
                        cnt["o"] += 1
                        if isq:
                            qknorm(P, nc, C, bi, gains[:, 32:33], C.eps_q[:], 1.0, qo[oi][:], ("n1qo", oi),
                                   qsq[qi], ("n1qs", qi), qrt[qi], ("n1qr", qi), 4 + qi)
                        else:
                            qknorm(P, nc, C, bi, gains[:, 33:34], C.eps6[:], 1.0 / 128, qo[oi][:], ("n1qo", oi),
                                   qsq[qi], ("n1qs", qi), qrt[qi], ("n1qr", qi), 4 + qi)
                        P.dma("sp", dst[head, :, t0 + th * 512:t0 + (th + 1) * 512], qo[oi][:],
                              reads=[("n1qo", oi)], writes=[("QK", isq, head, t0 + th * 512)])
            else:
                for tb in range(T // 128):
                    bi = cnt["ps"] % 4
                    cnt["ps"] += 1
                    ps = psbank(C, bi)

                    def mm(ps=ps, tb=tb, wb=wb):
                        for kc in range(KC):
                            ins = nc.tensor.matmul(ps, lhsT=hnT[:, kc, tb * 128:(tb + 1) * 128],
                                                   rhs=Wb[wb][:, kc, :], start=(kc == 0), stop=(kc == KC - 1))
                        return ins
                    P.op("pe", mm, reads=wkeys[si] + hreads, writes=[("ps", bi)])
                    oi = cnt["o"] % 4
                    cnt["o"] += 1
                    P.op("act", lambda ps=ps, oi=oi: nc.scalar.copy(out=qo[oi][:], in_=ps),
                         reads=[("ps", bi)], writes=[("n1qo", oi)])
                    P.dma("sp", Vv[:, t0 // 128 + tb, (g - 8) * 512:(g - 7) * 512], qo[oi][:],
                          reads=[("n1qo", oi)], writes=[("Vd", g - 8, t0 // 128 + tb)])
        P.barrier()


def stage_na2(P, nc, C, QT, KT, V, btab, ATs):
    sbt = mk_sbt(nc)
    with ExitStack() as st:
        Kb = [st.enter_context(sbt("n2K%d" % i, [128, TOKH], BF16)) for i in range(2)]
        Qb = [st.enter_context(sbt("n2Q%d" % i, [128, TOKH], BF16)) for i in range(2)]
        Vb = [st.enter_context(sbt("n2V%d" % i, [128, NSLOT, 128], BF16)) for i in range(2)]
        Bs = [st.enter_context(sbt("n2Bs%d" % i, [128, 3200], F32)) for i in range(2)]
        Bb = [st.enter_context(sbt("n2Bb%d" % i, [128, 5, 640], BF16)) for i in range(2)]
        PT = [st.enter_context(sbt("n2P%d" % i, [128, 640], BF16)) for i in range(3)]
        Ah = [st.enter_context(sbt("n2A%d" % i, [128, TOK], BF16)) for i in range(2)]
        rd = [st.enter_context(sbt("n2r%d" % i, [128, 128], F32)) for i in range(2)]
        Vv = V.rearrange("(c p) n -> p c n", p=128)
        cnt = 0

        def load_head(h):
            hb = h % 2
            for s0 in range(0, NSLOT, 17):
                s1 = min(NSLOT, s0 + 17)
                P.dma("sp", Vb[hb][:, s0:s1, :], Vv[:, s0:s1, h * 128:(h + 1) * 128], writes=[("n2V", hb, s0)])
            P.dma("sp", Kb[hb][:], KT[h], writes=[("n2K", hb)])
            P.dma("sp", Qb[hb][:], QT[h], writes=[("n2Q", hb)])
            P.dma("pool", Bb[hb][:].rearrange("p a b -> p (a b)"), btab[h], writes=[("n2Bb", hb)])

        load_head(0)
        for h in range(NHEAD):
            if h + 1 < NHEAD:
                load_head(h + 1)
            hb = h % 2
            vb = hb
            hv = 0
            for j in range(NBLK):
                ty = {0: 0, 1: 1, NBLK - 2: 3, NBLK - 1: 4}.get(j, 2)
                si = cnt % 2
                pi = cnt % 3
                cnt += 1
                S = C.psum[si][:, 0:640]
                skeys = [("ps", 2 * si), ("ps", 2 * si + 1)]

                def mmS(S=S, j=j, hb=hb, ty=ty):
                    for c in range(5):
                        nc.tensor.matmul(S[:, c * 128:(c + 1) * 128], lhsT=Kb[hb][:, (j + c) * 128:(j + c + 1) * 128],
                                         rhs=Qb[hb][:, (j + 2) * 128:(j + 3) * 128], start=True, stop=False)
                        ins = nc.tensor.matmul(S[:, c * 128:(c + 1) * 128], lhsT=C.ident_b[:],
                                               rhs=Bb[hb][:, ty, c * 128:(c + 1) * 128], start=False, stop=True)
                    return ins
                P.op("pe", mmS, reads=[("n2K", hb), ("n2Q", hb), ("n2Bb", hb)], writes=skeys)
                P.op("act", lambda S=S, pi=pi: nc.scalar.activation(out=PT[pi][:], in_=S, func=AF.Exp),
                     reads=skeys, writes=[("n2P", pi)])
                oi = 4 + si
                O = psbank(C, oi)

                def mmO(O=O, j=j, vb=vb, hv=hv, pi=pi):
                    for c in range(5):
                        nc.tensor.matmul(O[:, 0:128], lhsT=Vb[vb][:, j + c, hv * 128:(hv + 1) * 128],
                                         rhs=PT[pi][:, c * 128:(c + 1) * 128], start=(c == 0), stop=(c == 4))
                    for c in range(5):
                        ins = nc.tensor.matmul(O[:, 128:256], lhsT=C.ones_b[:],
                                               rhs=PT[pi][:, c * 128:(c + 1) * 128], start=(c == 0), stop=(c == 4))
                    return ins
                P.op("pe", mmO, reads=[("n2V", vb, s0) for s0 in range(0, NSLOT, 17)] + [("n2P", pi)], writes=[("ps", oi)])
                P.op("dve", lambda O=O, si=si: nc.vector.reciprocal(out=rd[si][:], in_=O[:, 128:256]),
                     reads=[("ps", oi)], writes=[("n2r", si)])
                P.op("dve", lambda O=O, si=si, j=j, hb=hb: nc.vector.tensor_tensor(
                    out=Ah[hb][:, j * 128:(j + 1) * 128], in0=O[:, 0:128], in1=rd[si][:], op=ALU.mult),
                    reads=[("ps", oi), ("n2r", si)], writes=[("n2A", hb)])
            P.dma("sp", ATs[h * 128:(h + 1) * 128, :], Ah[hb][:], reads=[("n2A", hb)], writes=[("ATs", h)])
        P.barrier()


def stage_out_mlp(P, nc, C, x_src, x_col0, projs, gain_mlp, w1, w2, outT, out_col0=0):
    sbt = mk_sbt(nc)
    T = 1024
    with ExitStack() as st:
        X = st.enter_context(sbt("mX", [128, KC, T], F32))
        hnT = st.enter_context(sbt("mh", [128, KC, T], BF16))
        W1b = [st.enter_context(sbt("mW1%d" % i, [128, KC, 512], BF16)) for i in range(2)]
        W2b = [st.enter_context(sbt("mW2%d" % i, [128, 4, D], BF16)) for i in range(2)]
        stg = [st.enter_context(sbt("ms%d" % i, [128, 2048], F32)) for i in range(2)]
        H = [st.enter_context(sbt("mH%d" % i, [128, 4, T], BF16)) for i in range(2)]
        rl = [st.enter_context(sbt("mr%d" % i, [128, 512], F32)) for i in range(2)]
        rtmp = st.enter_context(sbt("mrt", [128, 512], F32))
        wl = WLoader(P, nc, stg, "ms")
        xv = x_src.rearrange("(c p) t -> p c t", p=128)
        ov = outT.rearrange("(c p) t -> p c t", p=128)
        for ti in range(TOK // T):
            for c in range(KC):
                P.dma("sp", X[:, c, :], xv[:, c, x_col0 + ti * T:x_col0 + (ti + 1) * T], writes=[("X", c)])
            for (AT, w) in projs:
                av = AT.rearrange("(c p) t -> p c t", p=128)
                for c in range(KC):
                    P.dma("sp", hnT[:, c, :], av[:, c, ti * T:(ti + 1) * T], writes=[("hnT", c)])
                emit_proj_accum_tile(P, nc, C, X, hnT, "hnT", W1b, wl, w, KC, T, [4, 5, 6, 7])
            emit_rmsnorm_T(P, nc, C, X, "X", hnT, "hnT", gain_mlp, T, rl, "rl", rtmp, ("mrt",), 3)
            emit_mlp_tile(P, nc, C, X, hnT, W1b, W2b, H, rl, wl, w1, w2, T, [0, 1, 2], [4, 5, 6, 7])
            for c in range(KC):
                P.dma("sp", ov[:, c, out_col0 + ti * T:out_col0 + (ti + 1) * T], X[:, c, :], reads=[("X", c)], writes=[("out", c, ti)])
        P.barrier()


def setup_ctx(P, nc, st, cst):
    C = Ctx()
    sbt = mk_sbt(nc)
    C.psum = [st.enter_context(nc.psum_tensor("ps%d" % i, [128, 1024], F32)) for i in range(4)]
    C.ones_f = st.enter_context(sbt("ones_f", [128, 128], F32))
    C.ones_b = st.enter_context(sbt("ones_b", [128, 128], BF16))
    C.ident_f = st.enter_context(sbt("ident_f", [128, 128], F32))
    C.ident_b = st.enter_context(sbt("ident_b", [128, 128], BF16))
    C.eps6 = st.enter_context(sbt("eps6", [128, 1], F32))
    C.eps_q = st.enter_context(sbt("eps_q", [128, 1], F32))
    C.eps5 = st.enter_context(sbt("eps5", [128, 1], F32))
    P.op("pool", lambda: nc.gpsimd.memset(C.ones_f[:], 1.0))
    P.op("pool", lambda: nc.gpsimd.memset(C.ones_b[:], 1.0))
    P.op("pool", lambda: nc.gpsimd.memset(C.eps6[:], 1e-6))
    P.op("pool", lambda: nc.gpsimd.memset(C.eps_q[:], 128e-6))
    P.op("pool", lambda: nc.gpsimd.memset(C.eps5[:], 1e-5))
    P.dma("sp", C.ident_f[:], cst[:, 0:128], writes=["identf"])
    P.op("pool", lambda: nc.gpsimd.tensor_copy(out=C.ident_b[:], in_=C.ident_f[:]), reads=["identf"], writes=["identb"])
    return C


def build_phase_a():
    nc = bass.Bass("TRN2", target_bir_lowering=False)
    dt = nc.dram_tensor
    xhT = dt("xhT", [D, TOKH], F32, kind="ExternalInput").ap()
    gains = dt("gains", [128, 34], F32, kind="ExternalInput").ap()
    cst = dt("cst", [128, 384], F32, kind="ExternalInput").ap()
    wqkv = dt("wqkv", [D, 3 * D], F32, kind="ExternalInput").ap()
    wo = dt("wo", [D, D], F32, kind="ExternalInput").ap()
    w1 = dt("w1", [D, HID], F32, kind="ExternalInput").ap()
    w2 = dt("w2", [HID, D], F32, kind="ExternalInput").ap()
    btab = dt("btab", [NHEAD, 128, 3200], F32, kind="ExternalInput").ap()
    outT = dt("outT", [D, TOK], F32, kind="ExternalOutput").ap()
    QT = dt("QTs", [NHEAD, 128, TOKH], BF16, kind="Internal").ap()
    KT = dt("KTs", [NHEAD, 128, TOKH], BF16, kind="Internal").ap()
    V = dt("Vs", [TOKH, D], BF16, kind="Internal").ap()
    ATs = dt("ATs", [D, TOK], BF16, kind="Internal").ap()
    with ExitStack() as st:
        P = Prog(nc, st)
        C = setup_ctx(P, nc, st, cst)
        gsb = st.enter_context(nc.sbuf_tensor("gains_sb", [128, 34], F32))
        P.dma("sp", gsb[:], gains)
        P.barrier()
        stage_na1(P, nc, C, xhT, gsb, wqkv, QT, KT, V)
        stage_na2(P, nc, C, QT, KT, V, btab, ATs)
        stage_out_mlp(P, nc, C, xhT, HALO, [(ATs, wo)], gsb[:, 16:32], w1, w2, outT)
        P.finish()
        print("phase A: ops", P.n_ops, "waits", P.n_waits)
    return nc


def make_cst():
    c = np.zeros((128, 384), np.float32)
    c[:, 0:128] = np.eye(128, dtype=np.float32)
    c[:, 128:256] = np.triu(np.ones((128, 128), np.float32))
    c[:, 256:384] = np.tril(np.ones((128, 128), np.float32))
    return c


def pgain(g):
    return np.ascontiguousarray(g.reshape(KC, 128).T)


def slot_chunks(seg):
    R0c = NBLK * seg
    sl = []
    for s in range(NSLOT):
        g = R0c - 2 + s
        sl.append(g if 0 <= g < 64 else None)
    if seg == 0:
        sl[0] = 3
    if seg == NSEG - 1:
        sl[NSLOT - 1] = 60
    return sl


def na_halo_xT(xb, seg):
    sl = slot_chunks(seg)
    xh = np.zeros((TOKH, D), np.float32)
    for s, g in enumerate(sl):
        if g is not None:
            xh[s * 128:(s + 1) * 128] = xb[g * 128:(g + 1) * 128]
    return np.ascontiguousarray(xh.T)


def na_bias_tables(rel_bias, seg):
    sl = slot_chunks(seg)
    out = np.full((NHEAD, 128, 5, 5, 128), NEG, np.float32)
    kk = np.arange(128)
    qq = np.arange(128)
    for ty, j in enumerate([0, 1, 5, NBLK - 2, NBLK - 1]):
        gq = NBLK * seg + j
        r = 2 * gq + qq // 64
        xq = qq % 64
        r0 = np.clip(r - 4, 0, 120)
        c0 = np.clip(xq - 8, 0, 48)
        for c in range(5):
            gk = sl[j + c]
            if gk is None:
                continue
            rk = (2 * gk + kk // 64)[:, None]
            xk = (kk % 64)[:, None]
            valid = (rk >= r0[None, :]) & (rk < r0[None, :] + 8) & (xk >= c0[None, :]) & (xk < c0[None, :] + 16)
            ir = np.clip(rk - r[None, :] + 7, 0, 14)
            ic = np.clip(xk - xq[None, :] + 15, 0, 30)
            vals = rel_bias[:, ir, ic]
            out[:, :, ty, c, :] = np.where(valid[None], vals, np.float32(NEG))
    return np.ascontiguousarray(out.reshape(NHEAD, 128, 3200))


INNER = 4096
ICH = INNER // 128
TB1 = 1024
TB1H = TB1 + 3


def stage_b1(P, nc, C, xcT, gain, w_up, cwt, cbt, bd, wg, gb, qT, kT, vT, xcoT, zsT, gates, dbg=9):
    sbt = mk_sbt(nc)
    with ExitStack() as st:
        hnT = st.enter_context(sbt("b1h", [128, KC, TB1H], BF16))
        xk = [st.enter_context(sbt("b1x%d" % i, [128, TB1H], F32)) for i in range(2)]
        sq = [st.enter_context(sbt("b1q%d" % i, [128, TB1H], F32)) for i in range(2)]
        rt = st.enter_context(sbt("b1rt", [128, TB1H], F32))
        Wb = [st.enter_context(sbt("b1W%d" % i, [128, KC, 512], BF16)) for i in range(2)]
        stg = [st.enter_context(sbt("b1s%d" % i, [128, 2048], F32)) for i in range(4)]
        xm = [st.enter_context(sbt("b1xm%d" % i, [128, TB1H], F32)) for i in range(2)]
        acc = [st.enter_context(sbt("b1ac%d" % i, [128, TB1], F32)) for i in range(2)]
        xc = [st.enter_context(sbt("b1xc%d" % i, [128, TB1], BF16)) for i in range(2)]
        xmb = [st.enter_context(sbt("b1xb%d" % i, [128, TB1], BF16)) for i in range(2)]
        qo = [st.enter_context(sbt("b1qo%d" % i, [128, TB1], BF16)) for i in range(4)]
        zo = [st.enter_context(sbt("b1zo%d" % i, [128, TB1], BF16)) for i in range(2)]
        bds = [st.enter_context(sbt("b1bs%d" % i, [128, 3, 128], F32)) for i in range(2)]
        bdb = [st.enter_context(sbt("b1bb%d" % i, [128, 3, 128], BF16)) for i in range(2)]
        wgs = st.enter_context(sbt("b1wgs", [128, 96 * 16], F32))
        wgb = st.enter_context(sbt("b1wgb", [128, 96, 16], BF16))
        cw = st.enter_context(sbt("b1cw", [128, ICH, 4], F32))
        cb = st.enter_context(sbt("b1cb", [128, ICH], F32))
        gbs = st.enter_context(sbt("b1gb", [16, 1], F32))
        go = st.enter_context(sbt("b1go", [16, TB1], F32))
        wl = WLoader(P, nc, stg, "b1s")
        P.dma("sp", wgs[:], wg.rearrange("p a b -> p (a b)"), writes=["wgs"])
        P.op("pool", lambda: nc.gpsimd.tensor_copy(out=wgb[:].rearrange("p a b -> p (a b)"), in_=wgs[:]), reads=["wgs"], writes=["wgb"])
        P.dma("sp", cw[:], cwt, writes=["cw"])
        P.dma("sp", cb[:], cbt, writes=["cb"])
        P.dma("sp", gbs[:], gb, writes=["gb"])
        xv = xcT.rearrange("(c p) t -> p c t", p=128)
        wv = w_up.rearrange("(c p) n -> p c n", p=128)
        subs = [(0, 512), (512, 512), (1024, 3)]
        cnt = {"ps": 0, "hw": 0, "q": 0, "x": 0, "z": 0}
        for ti in range(TOK // TB1):
            c0 = ti * TB1
            for c in range(KC):
                xi = cnt["x"] % 2
                cnt["x"] += 1
                P.dma("sp", xk[xi][:], xv[:, c, c0:c0 + TB1H], writes=[("b1x", xi)])
                P.op("act", lambda xi=xi: nc.scalar.activation(out=sq[xi][:], in_=xk[xi][:], func=AF.Square),
                     reads=[("b1x", xi)], writes=[("b1q", xi)])
                for si, (s0, sn) in enumerate(subs):
                    ps = psbank(C, 4 + si)[:, 0:sn]
                    P.op("pe", lambda ps=ps, xi=xi, s0=s0, sn=sn, c=c: nc.tensor.matmul(
                        ps, lhsT=C.ones_f[:], rhs=sq[xi][:, s0:s0 + sn], start=(c == 0), stop=(c == KC - 1)),
                        reads=[("b1q", xi)], writes=[("ps", 4 + si)])
            for si, (s0, sn) in enumerate(subs):
                ps = psbank(C, 4 + si)[:, 0:sn]
                P.op("act", lambda ps=ps, s0=s0, sn=sn: nc.scalar.activation(
                    out=rt[:, s0:s0 + sn], in_=ps, func=AF.Sqrt, scale=1.0 / D, bias=C.eps6[:]),
                    reads=[("ps", 4 + si)], writes=[("b1rt", si)])
            P.op("dve", lambda: nc.vector.reciprocal(out=rt[:], in_=rt[:]),
                 reads=[("b1rt", i) for i in range(3)], writes=[("b1rt", i) for i in range(3)])
            for c in range(KC):
                xi = cnt["x"] % 2
                cnt["x"] += 1
                P.dma("sp", xk[xi][:], xv[:, c, c0:c0 + TB1H], writes=[("b1x", xi)])
                P.op("dve", lambda xi=xi, c=c: nc.vector.scalar_tensor_tensor(
                    out=hnT[:, c, :], in0=xk[xi][:], scalar=gain[:, c:c + 1], in1=rt[:], op0=ALU.mult, op1=ALU.mult),
                    reads=[("b1x", xi)] + [("b1rt", i) for i in range(3)], writes=[("hnT", c)])
            hreads = [("hnT", c) for c in range(KC)]
            if dbg < 2:
                continue
            wk_ = wl.load(wv, KC, 0, 512, Wb[0], ("b1W", 0))
            for g in range(16):
                wkeys = wk_
                if g + 1 < 16:
                    wk_ = wl.load(wv, KC, (g + 1) * 512, 512, Wb[(g + 1) % 2], ("b1W", (g + 1) % 2))
                wb = g % 2
                for i in range(4):
                    if g < 8:
                        cc = g * 4 + i
                        mi = cnt["hw"] % 2
                        P.dma("pool", bdb[mi][:], bd[cc], writes=[("b1bb", mi)])
                        for si, (s0, sn) in enumerate(subs):
                            bi = cnt["ps"] % 4
                            cnt["ps"] += 1
                            ps = psbank(C, bi)[:, 0:sn]

                            def mm(ps=ps, i=i, s0=s0, sn=sn, wb=wb):
                                for kc in range(KC):
                                    ins = nc.tensor.matmul(ps, lhsT=Wb[wb][:, kc, i * 128:(i + 1) * 128],
                                                           rhs=hnT[:, kc, s0:s0 + sn], start=(kc == 0), stop=(kc == KC - 1))
                                return ins
                            P.op("pe", mm, reads=wkeys + hreads, writes=[("ps", bi)])
                            P.op("act", lambda ps=ps, mi=mi, s0=s0, sn=sn: nc.scalar.copy(out=xm[mi][:, s0:s0 + sn], in_=ps),
                                 reads=[("ps", bi)], writes=[("b1xm", mi, si)])
                        xmk = [("b1xm", mi, si) for si in range(3)]
                        if dbg < 3:
                            cnt["hw"] += 1
                            continue
                        P.op("dve", lambda mi=mi, cc=cc: nc.vector.tensor_scalar(
                            out=acc[mi][:], in0=xm[mi][:, 0:TB1], scalar1=cw[:, cc, 0:1], scalar2=None, op0=ALU.mult),
                            reads=xmk + ["cw"], writes=[("b1ac", mi)])
                        for j in range(1, 4):
                            P.op("dve", lambda mi=mi, cc=cc, j=j: nc.vector.scalar_tensor_tensor(
                                out=acc[mi][:], in0=xm[mi][:, j:j + TB1], scalar=cw[:, cc, j:j + 1], in1=acc[mi][:],
                                op0=ALU.mult, op1=ALU.add),
                                reads=xmk + [("b1ac", mi)], writes=[("b1ac", mi)])
                        P.op("act", lambda mi=mi, cc=cc: nc.scalar.activation(
                            out=xc[mi][:], in_=acc[mi][:], func=AF.Silu, bias=cb[:, cc:cc + 1]),
                            reads=[("b1ac", mi), "cb"], writes=[("b1xc", mi)])
                        P.op("act", lambda mi=mi: nc.scalar.copy(out=xmb[mi][:], in_=xm[mi][:, 1:1 + TB1]),
                             reads=xmk, writes=[("b1xb", mi)])
                        P.dma("sp", xcoT[cc * 128:(cc + 1) * 128, c0:c0 + TB1], xc[mi][:], reads=[("b1xc", mi)],
                              writes=[("o_xc", cc, ti)])
                        cnt["hw"] += 1
                        if dbg < 4:
                            continue
                        for m, (src, skey, dst) in enumerate([(xc, "b1xc", qT), (xc, "b1xc", kT), (xmb, "b1xb", vT)]):
                            qi = cnt["q"] % 4
                            cnt["q"] += 1
                            for th in range(2):
                                bi = 6 + th
                                hb = cnt["ps"] % 4
                                cnt["ps"] += 1
                                ps = psbank(C, hb)
                                P.op("pe", lambda ps=ps, mi=mi, m=m, src=src, th=th: nc.tensor.matmul(
                                    ps, lhsT=bdb[mi][:, m, :], rhs=src[mi][:, th * 512:(th + 1) * 512], start=True, stop=True),
                                    reads=[("b1bb", mi), (skey, mi)], writes=[("ps", hb)])
                                if m >= 1:
                                    P.op("dve", lambda ps=ps, qi=qi, th=th: nc.vector.tensor_copy(
                                        out=qo[qi][:, th * 512:(th + 1) * 512], in_=ps),
                                        reads=[("ps", hb)], writes=[("b1qo", qi, th)])
                                else:
                                    P.op("act", lambda ps=ps, qi=qi, th=th: nc.scalar.copy(
                                        out=qo[qi][:, th * 512:(th + 1) * 512], in_=ps),
                                        reads=[("ps", hb)], writes=[("b1qo", qi, th)])
                                first = (cc == 0 and m == 0)
                                last = (cc == ICH - 1 and m == 2)
                                gps = psbank(C, bi)[0:16, :]
                                P.op("pe", lambda gps=gps, qi=qi, th=th, m=m, cc=cc, first=first, last=last: nc.tensor.matmul(
                                    gps, lhsT=wgb[:, m * 32 + cc, :], rhs=qo[qi][:, th * 512:(th + 1) * 512],
                                    start=first, stop=last),
                                    reads=[("b1qo", qi, th), "wgb"], writes=[("ps", bi)])
                            P.dma("sp", dst[cc * 128:(cc + 1) * 128, c0:c0 + TB1], qo[qi][:],
                                  reads=[("b1qo", qi, 0), ("b1qo", qi, 1)], writes=[("o_q", m, cc, ti)])
                    else:
                        if dbg < 5:
                            continue
                        cc = (g - 8) * 4 + i
                        zi = cnt["z"] % 2
                        cnt["z"] += 1
                        for th in range(2):
                            bi = cnt["ps"] % 4
                            cnt["ps"] += 1
                            ps = psbank(C, bi)

                            def mm(ps=ps, i=i, th=th, wb=wb):
                                for kc in range(KC):
                                    ins = nc.tensor.matmul(ps, lhsT=Wb[wb][:, kc, i * 128:(i + 1) * 128],
                                                           rhs=hnT[:, kc, 1 + th * 512:1 + (th + 1) * 512],
                                                           start=(kc == 0), stop=(kc == KC - 1))
                                return ins
                            P.op("pe", mm, reads=wkeys + hreads, writes=[("ps", bi)])
                            P.op("act", lambda ps=ps, zi=zi, th=th: nc.scalar.activation(
                                out=zo[zi][:, th * 512:(th + 1) * 512], in_=ps, func=AF.Silu),
                                reads=[("ps", bi)], writes=[("b1zo", zi, th)])
                        P.dma("sp", zsT[cc * 128:(cc + 1) * 128, c0:c0 + TB1], zo[zi][:],
                              reads=[("b1zo", zi, 0), ("b1zo", zi, 1)], writes=[("o_z", cc, ti)])
                if g == 7 and dbg >= 4:
                    for th in range(2):
                        gps = psbank(C, 6 + th)[0:16, :]
                        P.op("act", lambda gps=gps, th=th: nc.scalar.activation(
                            out=go[:, th * 512:(th + 1) * 512], in_=gps, func=AF.Identity, bias=gbs[:]),
                            reads=[("ps", 6 + th), "gb"], writes=[("b1go", th)])
                    P.dma("sp", gates[:, c0:c0 + TB1], go[:], reads=[("b1go", 0), ("b1go", 1)], writes=[("o_g", ti)])
        P.barrier()


def build_phase_b1(dbg=9):
    nc = bass.Bass("TRN2", target_bir_lowering=False)
    dt = nc.dram_tensor
    xcT = dt("xcT", [D, TOK + 3], F32, kind="ExternalInput").ap()
    gains = dt("gains", [128, KC], F32, kind="ExternalInput").ap()
    cst = dt("cst", [128, 384], F32, kind="ExternalInput").ap()
    w_up = dt("w_up", [D, 2 * INNER], F32, kind="ExternalInput").ap()
    cwt = dt("cwt", [128, ICH, 4], F32, kind="ExternalInput").ap()
    cbt = dt("cbt", [128, ICH], F32, kind="ExternalInput").ap()
    bd = dt("bd", [ICH, 128, 3, 128], F32, kind="ExternalInput").ap()
    wg = dt("wg", [128, 96, 16], F32, kind="ExternalInput").ap()
    gb = dt("gb", [16, 1], F32, kind="ExternalInput").ap()
    outs = {}
    for n in ["qT", "kT", "vT", "xcoT", "zsT"]:
        outs[n] = dt(n, [INNER, TOK], BF16, kind="ExternalOutput").ap()
    gates = dt("gates", [16, TOK], F32, kind="ExternalOutput").ap()
    with ExitStack() as st:
        P = Prog(nc, st)
        C = setup_ctx(P, nc, st, cst)
        gsb = st.enter_context(nc.sbuf_tensor("gains_sb", [128, KC], F32))
        P.dma("sp", gsb[:], gains)
        P.barrier()
        stage_b1(P, nc, C, xcT, gsb, w_up, cwt, cbt, bd, wg, gb, outs["qT"], outs["kT"], outs["vT"], outs["xcoT"], outs["zsT"], gates, dbg=dbg)
        P.finish()
        print("phase B1: ops", P.n_ops, "waits", P.n_waits)
    return nc


def b1_host_params(conv_w, conv_b, w_q, w_k, w_v, w_ig, b_ig, w_fg, b_fg):
    cwt = np.ascontiguousarray(conv_w.T.reshape(ICH, 128, 4).transpose(1, 0, 2))
    cbt = np.ascontiguousarray(conv_b.reshape(ICH, 128).T)
    bd = np.zeros((ICH, 128, 3, 128), np.float32)
    for m, w in enumerate([w_q, w_k, w_v]):
        wr = w.reshape(ICH, 32, 4, 4)
        for g in range(32):
            bd[:, 4 * g:4 * g + 4, m, 4 * g:4 * g + 4] = wr[:, g]
    W = np.concatenate([w_ig, w_fg], axis=1)
    wg = np.ascontiguousarray(W.reshape(3, ICH, 128, 16).transpose(2, 0, 1, 3).reshape(128, 96, 16))
    gb = np.concatenate([b_ig, b_fg]).reshape(16, 1).astype(np.float32)
    return dict(cwt=cwt, cbt=cbt, bd=bd, wg=wg, gb=gb)


SEQ = 8192
NCH = SEQ // 128
DH = 1024
DHC = DH // 128


def stage_b2_gates(P, nc, C, gin, grows, gsc, G):
    sbt = mk_sbt(nc)
    with ExitStack() as st:
        row = lambda n: st.enter_context(sbt(n, [1, SEQ], F32))
        zr = row("g_zr")
        ir = row("g_i")
        fr = row("g_f")
        t1 = row("g_t1")
        t2 = row("g_t2")
        mn = st.enter_context(sbt("g_mn", [1, NCH], F32))
        mp = st.enter_context(sbt("g_mp", [1, NCH], F32))
        one1 = C.ones_f[0:1, 0:1]
        P.op("pool", lambda: nc.gpsimd.memset(zr[:], 0.0), writes=["g_zr"])
        for dr in range(2):
            rev = (lambda t: t[:, ::-1]) if dr == 1 else (lambda t: t[:])
            ri, rf = grows[dr]
            P.dma("sp", t1[:], gin[ri:ri + 1, :], writes=["g_t1"])
            P.dma("sp", t2[:], gin[rf:rf + 1, :], writes=["g_t2"])
            P.op("dve", lambda: nc.vector.tensor_copy(out=ir[:], in_=rev(t1)), reads=["g_t1"], writes=["g_i"])
            P.op("dve", lambda: nc.vector.tensor_copy(out=fr[:], in_=rev(t2)), reads=["g_t2"], writes=["g_f"])
            P.op("act", lambda: nc.scalar.activation(out=fr[:], in_=fr[:], func=AF.Exp, scale=-1.0), reads=["g_f"], writes=["g_f"])
            P.op("act", lambda: nc.scalar.activation(out=fr[:], in_=fr[:], func=AF.Ln, bias=one1), reads=["g_f"], writes=["g_f"])
            P.op("dve", lambda: nc.vector.tensor_tensor_scan(out=t1[:], data0=fr[:], data1=zr[:], initial=0.0,
                                                             op0=ALU.add, op1=ALU.add),
                 reads=["g_f", "g_zr"], writes=["g_t1"])
            P.op("dve", lambda: nc.vector.tensor_tensor(out=ir[:], in0=ir[:], in1=t1[:], op=ALU.add),
                 reads=["g_i", "g_t1"], writes=["g_i"])
            P.op("dve", lambda: nc.vector.tensor_tensor_scan(out=t2[:], data0=ir[:], data1=ir[:], initial=0.0,
                                                             op0=ALU.max, op1=ALU.max),
                 reads=["g_i"], writes=["g_t2"])
            P.op("dve", lambda: nc.vector.tensor_copy(out=mn[:], in_=t2[:, 127::128]), reads=["g_t2"], writes=["g_mn"])
            P.op("pool", lambda: nc.gpsimd.memset(mp[:], 0.0), writes=["g_mp"])
            P.op("dve", lambda: nc.vector.tensor_copy(out=mp[:, 1:NCH], in_=mn[:, 0:NCH - 1]), reads=["g_mn", "g_mp"], writes=["g_mp"])
            P.op("dve", lambda: nc.vector.tensor_copy(out=fr[:], in_=rev(ir)), reads=["g_i", "g_f"], writes=["g_f"])
            P.dma("sp", gsc[dr, 0:1, :], fr[:], reads=["g_f"], writes=[("gsc", dr, 0)])
            P.op("dve", lambda: nc.vector.tensor_copy(out=t2[:], in_=rev(t1)), reads=["g_t1", "g_t2"], writes=["g_t2"])
            P.dma("sp", gsc[dr, 1:2, :], t2[:], reads=["g_t2"], writes=[("gsc", dr, 1)])
            P.op("dve", lambda: nc.vector.tensor_copy(out=t1[:, 0:NCH], in_=rev(mn)), reads=["g_mn", "g_t1"], writes=["g_t1"])
            P.op("dve", lambda: nc.vector.tensor_copy(out=t1[:, NCH:2 * NCH], in_=rev(mp)), reads=["g_mp", "g_t1"], writes=["g_t1"])
            P.dma("sp", gsc[dr, 2:3, 0:2 * NCH], t1[:, 0:2 * NCH], reads=["g_t1"], writes=[("gsc", dr, 2)])
            g = G[dr]
            P.dma("sp", g["A"][:], gsc[dr, 0, :].rearrange("(c p) -> p c", p=128), reads=[("gsc", dr, 0)],
                  writes=[("gA", dr)], allow_slow_non_contiguous=True)
            P.dma("sp", g["F"][:], gsc[dr, 1, :].rearrange("(c p) -> p c", p=128), reads=[("gsc", dr, 1)],
                  writes=[("gF", dr)], allow_slow_non_contiguous=True)
            P.dma("sp", g["MN"][:], gsc[dr, 2:3, 0:NCH].broadcast_to([128, NCH]), reads=[("gsc", dr, 2)], writes=[("gMN", dr)])
            P.dma("sp", g["MP"][:], gsc[dr, 2:3, NCH:2 * NCH].broadcast_to([128, NCH]), reads=[("gsc", dr, 2)], writes=[("gMP", dr)])
            for name, x, y in [("U", "A", "MP"), ("E2", "F", "MP"), ("WK", "A", "MN"), ("DEC", "MP", "MN")]:
                P.op("dve", lambda name=name, x=x, y=y: nc.vector.tensor_tensor(out=g[name][:], in0=g[x][:], in1=g[y][:], op=ALU.subtract),
                     reads=[("g" + x, dr), ("g" + y, dr)], writes=[("g" + name, dr)])
                P.op("act", lambda name=name: nc.scalar.activation(out=g[name][:], in_=g[name][:], func=AF.Exp),
                     reads=[("g" + name, dr)], writes=[("g" + name, dr)])
                if name in ("U", "WK"):
                    P.op("dve", lambda name=name: nc.vector.tensor_scalar(out=g[name][:], in0=g[name][:], scalar1=DH ** -0.5,
                                                                          scalar2=None, op0=ALU.mult),
                         reads=[("g" + name, dr)], writes=[("g" + name, dr)])
        P.barrier()


def stage_b2(P, nc, C, qTh, kTh, kt, vt, xct, zst, gin, onorm, skipv, cst, gsc, HFs, yt, nsteps=NCH, grows=((0, 1), (2, 3))):
    sbt = mk_sbt(nc)
    with ExitStack() as st:
        G = [{n: st.enter_context(sbt("G%d%s" % (dr, n), [128, NCH], F32)) for n in ["A", "F", "MN", "MP", "U", "E2", "WK", "DEC"]}
             for dr in range(2)]
        stage_b2_gates(P, nc, C, gin, grows, gsc, G)
        QT2 = [st.enter_context(sbt("s_q%d" % i, [128, DHC, 512], BF16)) for i in range(2)]
        KT2 = [st.enter_context(sbt("s_k%d" % i, [128, DHC, 512], BF16)) for i in range(2)]
        KTk = [st.enter_context(sbt("s_kt%d" % i, [128, 4, DH], BF16)) for i in range(2)]
        VTk = [st.enter_context(sbt("s_vt%d" % i, [128, 4, DH], BF16)) for i in range(2)]
        Cst = st.enter_context(sbt("s_C", [128, DHC, DH], F32))
        Cb = st.enter_context(sbt("s_Cb", [128, DHC, DH], BF16))
        nst = st.enter_context(sbt("s_n", [128, DHC], F32))
        nb = st.enter_context(sbt("s_nb", [128, DHC], BF16))
        SD = [st.enter_context(sbt("s_sd%d" % i, [128, 128], BF16)) for i in range(2)]
        wk = [st.enter_context(sbt("s_wk%d" % i, [128, DH], BF16)) for i in range(2)]
        hout = [st.enter_context(sbt("s_h%d" % i, [128, DH], F32)) for i in range(2)]
        rr = [st.enter_context(sbt("s_r%d" % i, [128, 1], F32)) for i in range(2)]
        tri = [st.enter_context(sbt("s_tri%d" % i, [128, 128], F32)) for i in range(2)]
        HF = [st.enter_context(sbt("s_hf%d" % i, [128, DH], F32)) for i in range(2)]
        XC = [st.enter_context(sbt("s_xc%d" % i, [128, DH], BF16)) for i in range(2)]
        ZS = [st.enter_context(sbt("s_zs%d" % i, [128, DH], BF16)) for i in range(2)]
        T1 = [st.enter_context(sbt("s_t1%d" % i, [128, DH], F32)) for i in range(2)]
        T3 = st.enter_context(sbt("s_t3", [128, DH], F32))
        Y = [st.enter_context(sbt("s_y%d" % i, [128, DH], BF16)) for i in range(2)]
        bst = st.enter_context(sbt("s_bst", [128, 2, 6], F32))
        mv = st.enter_context(sbt("s_mv", [128, 2], F32))
        rs = st.enter_context(sbt("s_rs", [128, 1], F32))
        on_sb = st.enter_context(sbt("s_on", [128, DH], F32))
        sk_sb = st.enter_context(sbt("s_sk", [128, DH], F32))
        P.dma("sp", tri[0][:], cst[:, 128:256], writes=[("tri", 0)])
        P.dma("sp", tri[1][:], cst[:, 256:384], writes=[("tri", 1)])
        P.dma("sp", on_sb[:], onorm, writes=["on"])
        P.dma("sp", sk_sb[:], skipv, writes=["sk"])
        qv = qTh.rearrange("(c p) t -> p c t", p=128)
        kv = kTh.rearrange("(c p) t -> p c t", p=128)
        ktv = kt.rearrange("(c p) d -> p c d", p=128)
        vtv = vt.rearrange("(c p) d -> p c d", p=128)
        Ckeys = [("C", dc, eh) for dc in range(DHC) for eh in range(2)]
        Cbkeys = [("Cb", dc, eh) for dc in range(DHC) for eh in range(2)]
        cnt = {"dc": 0, "cast": 0}

        def load_super(sc, buf):
            P.dma("sp", QT2[buf][:], qv[:, :, sc * 512:(sc + 1) * 512], writes=[("s_q", buf)])
            P.dma("sp", KT2[buf][:], kv[:, :, sc * 512:(sc + 1) * 512], writes=[("s_k", buf)])
            P.dma("sp", KTk[buf][:], ktv[:, sc * 4:(sc + 1) * 4, :], writes=[("s_kt", buf)])
            P.dma("sp", VTk[buf][:], vtv[:, sc * 4:(sc + 1) * 4, :], writes=[("s_vt", buf)])

        for dr in (range(2) if nsteps == NCH else range(1)):
            g = G[dr]
            P.op("pool", lambda: nc.gpsimd.memset(Cst[:], 0.0), writes=Ckeys)
            P.op("pool", lambda: nc.gpsimd.memset(Cb[:], 0.0), writes=Cbkeys)
            P.op("pool", lambda: nc.gpsimd.memset(nst[:], 0.0), writes=["n"])
            P.op("pool", lambda: nc.gpsimd.memset(nb[:], 0.0), writes=["nb"])
            order = list(range(NCH)) if dr == 0 else list(range(NCH - 1, -1, -1))
            order = order[:nsteps]
            scs = []
            for c in order:
                if not scs or scs[-1] != c // 4:
                    scs.append(c // 4)
            sbuf_of = {sc: i % 2 for i, sc in enumerate(scs)}
            load_super(scs[0], 0)
            for k, c in enumerate(order):
                sc = c // 4
                lc = c % 4
                buf = sbuf_of[sc]
                first_in_sc = (k == 0) or (order[k - 1] // 4 != sc)
                if first_in_sc:
                    i = scs.index(sc)
                    if i + 1 < len(scs):
                        load_super(scs[i + 1], (i + 1) % 2)
                par = k % 2
                tsl = slice(lc * 128, (lc + 1) * 128)
                if dr == 1:
                    P.dma("sp", HF[par][:], HFs[c * 128:(c + 1) * 128, :], reads=[("HFs", c)], writes=[("s_hf", par)])
                    P.dma("sp", XC[par][:], xct[c * 128:(c + 1) * 128, :], writes=[("s_xc", par)])
                    P.dma("sp", ZS[par][:], zst[c * 128:(c + 1) * 128, :], writes=[("s_zs", par)])
                sA = psbank(C, 0)[:, par * 128:(par + 1) * 128]

                def mmS(sA=sA, buf=buf, tsl=tsl):
                    for dc in range(DHC):
                        ins = nc.tensor.matmul(sA, lhsT=KT2[buf][:, dc, tsl], rhs=QT2[buf][:, dc, tsl],
                                               start=(dc == 0), stop=(dc == DHC - 1))
                    return ins
                P.op("pe", mmS, reads=[("s_k", buf), ("s_q", buf)], writes=[("psS", par)])
                P.op("dve", lambda sA=sA, par=par, c=c, dr=dr: nc.vector.scalar_tensor_tensor(
                    out=SD[par][:], in0=sA, scalar=g["U"][:, c:c + 1], in1=tri[dr][:], op0=ALU.mult, op1=ALU.mult),
                    reads=[("psS", par), ("gU", dr), ("tri", dr)], writes=[("s_sd", par)])
                for eh in range(2):
                    nb_ = 2 + eh
                    ps = psbank(C, nb_)

                    def mmN(ps=ps, eh=eh, par=par, buf=buf, lc=lc, tsl=tsl):
                        nc.tensor.matmul(ps, lhsT=SD[par][:], rhs=VTk[buf][:, lc, eh * 512:(eh + 1) * 512], start=True, stop=False)
                        for dc in range(DHC):
                            ins = nc.tensor.matmul(ps, lhsT=QT2[buf][:, dc, tsl], rhs=Cb[:, dc, eh * 512:(eh + 1) * 512],
                                                   start=False, stop=(dc == DHC - 1))
                        return ins
                    P.op("pe", mmN, reads=[("s_sd", par), ("s_vt", buf), ("s_q", buf)] + [("Cb", dc, eh) for dc in range(DHC)],
                         writes=[("ps", nb_)])
                dn = psbank(C, 1)[:, par:par + 1]

                def mmD(dn=dn, par=par, buf=buf, tsl=tsl):
                    nc.tensor.matmul(dn, lhsT=SD[par][:], rhs=C.ones_b[:, 0:1], start=True, stop=False)
                    for dc in range(DHC):
                        ins = nc.tensor.matmul(dn, lhsT=QT2[buf][:, dc, tsl], rhs=nb[:, dc:dc + 1], start=False, stop=(dc == DHC - 1))
                    return ins
                P.op("pe", mmD, reads=[("s_sd", par), ("s_q", buf), "nb"], writes=[("ps4", par)])
                P.op("act", lambda dn=dn, par=par: nc.scalar.activation(out=rr[par][:], in_=dn, func=AF.Abs),
                     reads=[("ps4", par)], writes=[("s_r", par)])
                P.op("dve", lambda par=par, c=c: nc.vector.tensor_scalar(
                    out=rr[par][:], in0=rr[par][:], scalar1=g["E2"][:, c:c + 1], scalar2=None, op0=ALU.max),
                    reads=[("s_r", par), ("gE2", dr)], writes=[("s_r", par)])
                P.op("dve", lambda par=par: nc.vector.reciprocal(out=rr[par][:], in_=rr[par][:]), reads=[("s_r", par)], writes=[("s_r", par)])
                P.op("act", lambda par=par: nc.scalar.activation(
                    out=hout[par][:], in_=C.psum[1][:], func=AF.Copy, scale=rr[par][:, 0:1]),
                    reads=[("ps", 2), ("ps", 3), ("s_r", par)], writes=[("s_h", par, 0), ("s_h", par, 1)])
                hk = [("s_h", par, 0), ("s_h", par, 1)]
                if dr == 0:
                    P.dma("sp", HFs[c * 128:(c + 1) * 128, :], hout[par][:], reads=hk, writes=[("HFs", c)])
                else:
                    t1 = T1[par]
                    P.op("dve", lambda par=par, t1=t1: nc.vector.tensor_tensor(out=t1[:], in0=hout[par][:], in1=HF[par][:], op=ALU.add),
                         reads=hk + [("s_hf", par)], writes=[("s_t1", par)])
                    for hh in range(2):
                        P.op("dve", lambda hh=hh, t1=t1: nc.vector.bn_stats(out=bst[:, hh, :], in_=t1[:, hh * 512:(hh + 1) * 512]),
                             reads=[("s_t1", par)], writes=[("bst", hh)])
                    P.op("dve", lambda: nc.vector.bn_aggr(out=mv[:], in_=bst[:].rearrange("p a b -> p (a b)")),
                         reads=[("bst", 0), ("bst", 1)], writes=["mv"])
                    P.op("act", lambda: nc.scalar.activation(out=rs[:], in_=mv[:, 1:2], func=AF.Sqrt, bias=C.eps5[:]),
                         reads=["mv"], writes=["rs"])
                    P.op("dve", lambda: nc.vector.reciprocal(out=rs[:], in_=rs[:]), reads=["rs"], writes=["rs"])
                    P.op("dve", lambda t1=t1: nc.vector.tensor_scalar(out=t1[:], in0=t1[:], scalar1=mv[:, 0:1], scalar2=rs[:, 0:1],
                                                                      op0=ALU.subtract, op1=ALU.mult),
                         reads=[("s_t1", par), "mv", "rs"], writes=[("s_t1", par)])
                    P.op("dve", lambda t1=t1: nc.vector.tensor_tensor(out=t1[:], in0=t1[:], in1=on_sb[:], op=ALU.mult),
                         reads=[("s_t1", par), "on"], writes=[("s_t1", par)])
                    P.op("pool", lambda par=par: nc.gpsimd.tensor_tensor(out=T3[:], in0=XC[par][:], in1=sk_sb[:], op=ALU.mult),
                         reads=[("s_xc", par), "sk"], writes=["s_t3"])
                    P.op("dve", lambda t1=t1: nc.vector.tensor_tensor(out=t1[:], in0=t1[:], in1=T3[:], op=ALU.add),
                         reads=[("s_t1", par), "s_t3"], writes=[("s_t1", par)])
                    P.op("pool", lambda par=par, t1=t1: nc.gpsimd.tensor_tensor(out=Y[par][:], in0=t1[:], in1=ZS[par][:], op=ALU.mult),
                         reads=[("s_t1", par), ("s_zs", par)], writes=[("s_y", par)])
                    P.dma("sp", yt[c * 128:(c + 1) * 128, :], Y[par][:], reads=[("s_y", par)], writes=[("yt", c)])
                P.op("act", lambda par=par, buf=buf, lc=lc, c=c: nc.scalar.activation(
                    out=wk[par][:], in_=KTk[buf][:, lc, :], func=AF.Copy, scale=g["WK"][:, c:c + 1]),
                    reads=[("s_kt", buf), ("gWK", dr)], writes=[("s_wk", par)])
                for dc in range(DHC):
                    gi = 2 + cnt["dc"] % 2
                    cnt["dc"] += 1
                    pt = C.psum[gi]

                    def mmC(pt=pt, par=par, dc=dc, buf=buf, lc=lc):
                        for eh in range(2):
                            ins = nc.tensor.matmul(pt[:, eh * 512:(eh + 1) * 512], lhsT=wk[par][:, dc * 128:(dc + 1) * 128],
                                                   rhs=VTk[buf][:, lc, eh * 512:(eh + 1) * 512], start=True, stop=True)
                        return ins
                    P.op("pe", mmC, reads=[("s_wk", par), ("s_vt", buf)], writes=[("ps", 2 * gi), ("ps", 2 * gi + 1)])
                    P.op("dve", lambda pt=pt, dc=dc, c=c: nc.vector.scalar_tensor_tensor(
                        out=Cst[:, dc, :], in0=Cst[:, dc, :], scalar=g["DEC"][:, c:c + 1], in1=pt[:], op0=ALU.mult, op1=ALU.add),
                        reads=[("ps", 2 * gi), ("ps", 2 * gi + 1), ("C", dc, 0), ("C", dc, 1), ("gDEC", dr)],
                        writes=[("C", dc, 0), ("C", dc, 1)])
                    P.op("act", lambda dc=dc: nc.scalar.copy(out=Cb[:, dc, :], in_=Cst[:, dc, :]),
                         reads=[("C", dc, 0), ("C", dc, 1)], writes=[("Cb", dc, 0), ("Cb", dc, 1)])
                dnn = psbank(C, 1)[:, 8 + 8 * par:16 + 8 * par]

                def mmn(dnn=dnn, par=par):
                    for dc in range(DHC):
                        ins = nc.tensor.matmul(dnn[:, dc:dc + 1], lhsT=wk[par][:, dc * 128:(dc + 1) * 128], rhs=C.ones_b[:, 0:1],
                                               start=True, stop=True)
                    return ins
                P.op("pe", mmn, reads=[("s_wk", par)], writes=[("ps4n", par)])
                P.op("dve", lambda dnn=dnn, c=c: nc.vector.scalar_tensor_tensor(
                    out=nst[:], in0=nst[:], scalar=g["DEC"][:, c:c + 1], in1=dnn, op0=ALU.mult, op1=ALU.add),
                    reads=[("ps4n", par), "n", ("gDEC", dr)], writes=["n"])
                P.op("dve", lambda: nc.vector.tensor_copy(out=nb[:], in_=nst[:]), reads=["n"], writes=["nb"])
        P.barrier()


def build_phase_b2(nsteps=NCH):
    nc = bass.Bass("TRN2", target_bir_lowering=False)
    dt = nc.dram_tensor
    qTh = dt("qTh", [DH, SEQ], BF16, kind="ExternalInput").ap()
    kTh = dt("kTh", [DH, SEQ], BF16, kind="ExternalInput").ap()
    kt = dt("kt", [SEQ, DH], BF16, kind="ExternalInput").ap()
    vt = dt("vt", [SEQ, DH], BF16, kind="ExternalInput").ap()
    xct = dt("xct", [SEQ, DH], BF16, kind="ExternalInput").ap()
    zst = dt("zst", [SEQ, DH], BF16, kind="ExternalInput").ap()
    gin = dt("gin", [4, SEQ], F32, kind="ExternalInput").ap()
    onorm = dt("onorm", [128, DH], F32, kind="ExternalInput").ap()
    skipv = dt("skipv", [128, DH], F32, kind="ExternalInput").ap()
    cst = dt("cst", [128, 384], F32, kind="ExternalInput").ap()
    yt = dt("yt", [SEQ, DH], BF16, kind="ExternalOutput").ap()
    gsc = dt("gsc", [2, 3, SEQ], F32, kind="Internal").ap()
    HFs = dt("HFs", [SEQ, DH], F32, kind="Internal").ap()
    with ExitStack() as st:
        P = Prog(nc, st)
        C = setup_ctx(P, nc, st, cst)
        P.barrier()
        stage_b2(P, nc, C, qTh, kTh, kt, vt, xct, zst, gin, onorm, skipv, cst, gsc, HFs, yt, nsteps=nsteps)
        P.finish()
        print("phase B2: ops", P.n_ops, "waits", P.n_waits)
    return nc


def build_phase_b3():
    nc = bass.Bass("TRN2", target_bir_lowering=False)
    dt = nc.dram_tensor
    xT = dt("xT", [D, TOK], F32, kind="ExternalInput").ap()
    yT = dt("yT", [INNER, TOK], BF16, kind="ExternalInput").ap()
    gains = dt("gains", [128, KC], F32, kind="ExternalInput").ap()
    cst = dt("cst", [128, 384], F32, kind="ExternalInput").ap()
    wd = dt("wd", [INNER, D], F32, kind="ExternalInput").ap()
    w1 = dt("w1", [D, HID], F32, kind="ExternalInput").ap()
    w2 = dt("w2", [HID, D], F32, kind="ExternalInput").ap()
    outT = dt("outT", [D, TOK], F32, kind="ExternalOutput").ap()
    with ExitStack() as st:
        P = Prog(nc, st)
        C = setup_ctx(P, nc, st, cst)
        gsb = st.enter_context(nc.sbuf_tensor("gains_sb", [128, KC], F32))
        P.dma("sp", gsb[:], gains)
        P.barrier()
        stage_out_mlp(P, nc, C, xT, 0, [(yT[0:D, :], wd[0:D, :]), (yT[D:2 * D, :], wd[D:2 * D, :])], gsb, w1, w2, outT)
        P.finish()
        print("phase B3: ops", P.n_ops, "waits", P.n_waits)
    return nc


_PROGS = {}


def _prog(name):
    if name not in _PROGS:
        _PROGS[name] = {"A": build_phase_a, "B1": build_phase_b1, "B2": build_phase_b2, "B3": build_phase_b3}[name]()
    return _PROGS[name]


def _run(name, in_maps):
    res = run_bass_kernel_spmd(_prog(name), in_maps, core_ids=list(range(NCORE)))
    return res.results


def kernel_unfused_8core(x, norm_mix, norm_mlp, na_w_qkv, na_q_gain, na_k_gain, na_rel_bias, na_w_o,
           ml_w_up, ml_conv_w, ml_conv_b, ml_w_q, ml_w_k, ml_w_v, ml_w_ig, ml_b_ig,
           ml_w_fg, ml_b_fg, ml_out_norm, ml_skip, ml_w_down, mlp_w1, mlp_w2):
    f32 = lambda a: np.ascontiguousarray(np.asarray(a, dtype=np.float32))
    x = f32(x)
    cst = make_cst()
    B = 2
    xs = [x[b] for b in range(B)]
    xT_core = None
    for layer in range(4):
        j = layer // 2
        w1, w2 = f32(mlp_w1[layer]), f32(mlp_w2[layer])
        if layer % 2 == 0:
            gains = f32(np.concatenate([pgain(f32(norm_mix[layer])), pgain(f32(norm_mlp[layer])),
                                        f32(na_q_gain[j])[:, None], f32(na_k_gain[j])[:, None]], axis=1))
            wqkv, wo = f32(na_w_qkv[j]), f32(na_w_o[j])
            rb = f32(na_rel_bias[j])
            btabs = [na_bias_tables(rb, seg) for seg in range(4)]
            ims = []
            for core in range(NCORE):
                b, seg = core // 4, core % 4
                ims.append({"xhT": na_halo_xT(xs[b], seg), "gains": gains, "cst": cst, "wqkv": wqkv, "wo": wo,
                            "w1": w1, "w2": w2, "btab": btabs[seg]})
            res = _run("A", ims)
            xT_core = [r["outT"] for r in res]
        else:
            hp = b1_host_params(f32(ml_conv_w[j]), f32(ml_conv_b[j]), f32(ml_w_q[j]), f32(ml_w_k[j]), f32(ml_w_v[j]),
                                f32(ml_w_ig[j]), f32(ml_b_ig[j]), f32(ml_w_fg[j]), f32(ml_b_fg[j]))
            gmix = pgain(f32(norm_mix[layer]))
            wup = f32(ml_w_up[j])
            ims = []
            for core in range(NCORE):
                b, seg = core // 4, core % 4
                xc = np.zeros((TOK + 3, D), np.float32)
                lo, hi = seg * TOK - 1, seg * TOK + TOK + 2
                a, e = max(lo, 0), min(hi, SEQ)
                xc[a - lo:e - lo] = xs[b][a:e]
                ims.append({"xcT": np.ascontiguousarray(xc.T), "gains": gmix, "cst": cst, "w_up": wup, **hp})
            r1 = _run("B1", ims)
            on, sk = f32(ml_out_norm[j]), f32(ml_skip[j])
            ims = []
            for core in range(NCORE):
                b, h = core // 4, core % 4
                hs = slice(h * DH, (h + 1) * DH)
                cat = lambda n: np.concatenate([r1[b * 4 + s][n][hs, :] for s in range(4)], axis=1)
                qTh, kTh, vTh, xcTh, zsTh = cat("qT"), cat("kT"), cat("vT"), cat("xcoT"), cat("zsT")
                gfull = np.concatenate([r1[b * 4 + s]["gates"] for s in range(4)], axis=1)
                gin = np.ascontiguousarray(gfull[[h, 8 + h, 4 + h, 12 + h], :])
                ims.append({"qTh": np.ascontiguousarray(qTh), "kTh": np.ascontiguousarray(kTh),
                            "kt": np.ascontiguousarray(kTh.T), "vt": np.ascontiguousarray(vTh.T),
                            "xct": np.ascontiguousarray(xcTh.T), "zst": np.ascontiguousarray(zsTh.T),
                            "gin": gin, "onorm": np.ascontiguousarray(np.tile(on[hs], (128, 1))),
                            "skipv": np.ascontiguousarray(np.tile(sk[hs], (128, 1))), "cst": cst})
            r2 = _run("B2", ims)
            gmlp = pgain(f32(norm_mlp[layer]))
            wd = f32(ml_w_down[j])
            ims = []
            for core in range(NCORE):
                b, seg = core // 4, core % 4
                ts = slice(seg * TOK, (seg + 1) * TOK)
                yT = np.concatenate([r2[b * 4 + h]["yt"][ts, :].T for h in range(4)], axis=0)
                ims.append({"xT": xT_core[core], "yT": np.ascontiguousarray(yT), "gains": gmlp, "cst": cst,
                            "wd": wd, "w1": w1, "w2": w2})
            res = _run("B3", ims)
            xT_core = [r["outT"] for r in res]
        xs = [np.concatenate([xT_core[b * 4 + s].T for s in range(4)], axis=0) for b in range(B)]
    return np.ascontiguousarray(np.stack(xs, axis=0).astype(np.float32))


def stage_transpose(P, nc, C, src, dst, R, Cc):
    sbt = mk_sbt(nc)
    with ExitStack() as st:
        S = [st.enter_context(sbt("tS%d" % i, [128, 8, 512], BF16)) for i in range(2)]
        O = [st.enter_context(sbt("tO%d" % i, [128, 1024], BF16)) for i in range(4)]
        sv = src.rearrange("(c p) n -> p c n", p=128)
        it = 0
        oc = 0
        for rg in range(R // 1024):
            for cg in range(Cc // 512):
                sb = it % 2
                it += 1
                P.dma("sp", S[sb][:], sv[:, rg * 8:(rg + 1) * 8, cg * 512:(cg + 1) * 512], writes=[("tS", sb)])
                for q in range(4):
                    bi = oc % 4
                    oi = oc % 4
                    oc += 1
                    pv = psbank(C, bi).bitcast(BF16)

                    def tr(pv=pv, sb=sb, q=q):
                        for rb in range(8):
                            ins = nc.tensor.transpose(out=pv[:, rb * 128:(rb + 1) * 128], in_=S[sb][:, rb, q * 128:(q + 1) * 128],
                                                      identity=C.ident_b[:])
                        return ins
                    P.op("pe", tr, reads=[("tS", sb)], writes=[("ps", bi)])
                    if oc % 2 == 0:
                        P.op("act", lambda pv=pv, oi=oi: nc.scalar.copy(out=O[oi][:], in_=pv), reads=[("ps", bi)], writes=[("tO", oi)])
                    else:
                        P.op("dve", lambda pv=pv, oi=oi: nc.vector.tensor_copy(out=O[oi][:], in_=pv), reads=[("ps", bi)], writes=[("tO", oi)])
                    c0 = cg * 512 + q * 128
                    P.dma("sp", dst[c0:c0 + 128, rg * 1024:(rg + 1) * 1024], O[oi][:], reads=[("tO", oi)], writes=[("tD", c0, rg)])
        P.barrier()


def build_fused(layers=4):
    nc = bass.Bass("TRN2", target_bir_lowering=False)
    dt = nc.dram_tensor
    ein = lambda n, shp, t=F32: dt(n, shp, t, kind="ExternalInput").ap()
    scr = lambda n, shp, t: dt(n, shp, t, kind="Internal").ap()
    xhT = ein("xhT", [D, TOKH])
    cst = ein("cst", [128, 384])
    gna = ein("gna", [2, 128, 34])
    gml = ein("gml", [2, 128, 32])
    wqkv = ein("wqkv", [2, D, 3 * D])
    wo = ein("wo", [2, D, D])
    btab = ein("btab", [2, NHEAD, 128, 3200])
    w1 = ein("w1", [4, D, HID])
    w2 = ein("w2", [4, HID, D])
    w_up = ein("w_up", [2, D, 2 * INNER])
    cwt = ein("cwt", [2, 128, ICH, 4])
    cbt = ein("cbt", [2, 128, ICH])
    bd = ein("bd", [2, ICH, 128, 3, 128])
    wg = ein("wg", [2, 128, 96, 16])
    gb = ein("gb", [2, 16, 1])
    onb = ein("onb", [2, 4, 128, DH])
    skb = ein("skb", [2, 4, 128, DH])
    wd = ein("wd", [2, INNER, D])
    outT = dt("outT", [D, TOK], F32, kind="ExternalOutput").ap()
    XS = scr("XS", [D, TOKH], F32)
    QT = scr("QTs", [NHEAD, 128, TOKH], BF16)
    KT = scr("KTs", [NHEAD, 128, TOKH], BF16)
    V = scr("Vs", [TOKH, D], BF16)
    ATs = scr("ATs", [D, TOK], BF16)
    fm = {n: scr(n, [INNER, TOK], BF16) for n in ["qT", "kT", "vT", "xcoT", "zsT"]}
    tm = {n: scr(n, [TOK, INNER], BF16) for n in ["kt", "vt", "xct", "zst", "yt"]}
    yT = scr("yT", [INNER, TOK], BF16)
    gates = scr("gates", [16, TOK], F32)
    gsc = scr("gsc", [2, 3, SEQ], F32)
    HFs = scr("HFs", [SEQ, DH], F32)
    with ExitStack() as st:
        P = Prog(nc, st)
        C = setup_ctx(P, nc, st, cst)
        gna_sb = [st.enter_context(nc.sbuf_tensor("gna%d" % i, [128, 34], F32)) for i in range(2)]
        gml_sb = [st.enter_context(nc.sbuf_tensor("gml%d" % i, [128, 32], F32)) for i in range(2)]
        for i in range(2):
            P.dma("sp", gna_sb[i][:], gna[i])
            P.dma("sp", gml_sb[i][:], gml[i])
        for c in range(KC):
            P.dma("sp", XS[c * 128:(c + 1) * 128, :], xhT[c * 128:(c + 1) * 128, :])
        P.barrier()
        for layer in range(layers):
            j = layer // 2
            last = (layer == layers - 1)
            dst, dcol = (outT, 0) if last else (XS, HALO)
            if layer % 2 == 0:
                stage_na1(P, nc, C, XS, gna_sb[j], wqkv[j], QT, KT, V)
                stage_na2(P, nc, C, QT, KT, V, btab[j], ATs)
                stage_out_mlp(P, nc, C, XS, HALO, [(ATs, wo[j])], gna_sb[j][:, 16:32], w1[layer], w2[layer], dst, dcol)
            else:
                stage_b1(P, nc, C, XS[:, HALO - 1:HALO + TOK + 2], gml_sb[j][:, 0:16], w_up[j], cwt[j], cbt[j], bd[j], wg[j], gb[j],
                         fm["qT"], fm["kT"], fm["vT"], fm["xcoT"], fm["zsT"], gates)
                for a, b in [("kT", "kt"), ("vT", "vt"), ("xcoT", "xct"), ("zsT", "zst")]:
                    stage_transpose(P, nc, C, fm[a], tm[b], INNER, TOK)
                for h in range(4):
                    hs = slice(h * DH, (h + 1) * DH)
                    stage_b2(P, nc, C, fm["qT"][hs, :], fm["kT"][hs, :], tm["kt"][:, hs], tm["vt"][:, hs], tm["xct"][:, hs],
                             tm["zst"][:, hs], gates, onb[j, h], skb[j, h], cst, gsc, HFs, tm["yt"][:, hs],
                             grows=((h, 8 + h), (4 + h, 12 + h)))
                stage_transpose(P, nc, C, tm["yt"], yT, TOK, INNER)
                stage_out_mlp(P, nc, C, XS, HALO, [(yT[0:D, :], wd[j, 0:D, :]), (yT[D:2 * D, :], wd[j, D:2 * D, :])],
                              gml_sb[j][:, 16:32], w1[layer], w2[layer], dst, dcol)
                if not last:
                    for c in range(KC):
                        rs_ = slice(c * 128, (c + 1) * 128)
                        P.dma("sp", XS[rs_, 0:128], XS[rs_, HALO + 3 * 128:HALO + 4 * 128])
                        P.dma("sp", XS[rs_, TOKH - 128:TOKH], XS[rs_, HALO + 60 * 128:HALO + 61 * 128])
                    P.barrier()
        P.finish()
        print("fused: ops", P.n_ops, "waits", P.n_waits)
    return nc


def kernel(x, norm_mix, norm_mlp, na_w_qkv, na_q_gain, na_k_gain, na_rel_bias, na_w_o,
           ml_w_up, ml_conv_w, ml_conv_b, ml_w_q, ml_w_k, ml_w_v, ml_w_ig, ml_b_ig,
           ml_w_fg, ml_b_fg, ml_out_norm, ml_skip, ml_w_down, mlp_w1, mlp_w2):
    f32 = lambda a: np.ascontiguousarray(np.asarray(a, dtype=np.float32))
    x = f32(x)
    norm_mix, norm_mlp = f32(norm_mix), f32(norm_mlp)
    gna = np.stack([np.concatenate([pgain(norm_mix[2 * j]), pgain(norm_mlp[2 * j]),
                                    f32(na_q_gain[j])[:, None], f32(na_k_gain[j])[:, None]], axis=1) for j in range(2)])
    gml = np.stack([np.concatenate([pgain(norm_mix[2 * j + 1]), pgain(norm_mlp[2 * j + 1])], axis=1) for j in range(2)])
    hps = [b1_host_params(f32(ml_conv_w[j]), f32(ml_conv_b[j]), f32(ml_w_q[j]), f32(ml_w_k[j]), f32(ml_w_v[j]),
                          f32(ml_w_ig[j]), f32(ml_b_ig[j]), f32(ml_w_fg[j]), f32(ml_b_fg[j])) for j in range(2)]
    stk = lambda k: np.ascontiguousarray(np.stack([hps[j][k] for j in range(2)]))
    on, sk = f32(ml_out_norm), f32(ml_skip)
    rep = lambda v: np.ascontiguousarray(np.stack([np.stack([np.tile(v[j, h * DH:(h + 1) * DH], (128, 1)) for h in range(4)])
                                                   for j in range(2)]))
    common = {
        "cst": make_cst(), "gna": f32(gna), "gml": f32(gml), "wqkv": f32(na_w_qkv), "wo": f32(na_w_o),
        "btab": np.ascontiguousarray(np.stack([na_bias_tables(f32(na_rel_bias[j]), 0) for j in range(2)])),
        "w1": f32(mlp_w1), "w2": f32(mlp_w2), "w_up": f32(ml_w_up),
        "cwt": stk("cwt"), "cbt": stk("cbt"), "bd": stk("bd"), "wg": stk("wg"), "gb": stk("gb"),
        "onb": rep(on), "skb": rep(sk), "wd": f32(ml_w_down),
    }
    ims = [dict(common, xhT=na_halo_xT(x[b], 0)) for b in range(NCORE)]
    if "fused" not in _PROGS:
        _PROGS["fused"] = build_fused()
    res = run_bass_kernel_spmd(_PROGS["fused"], ims, core_ids=list(range(NCORE)))
    out = np.stack([res.results[b]["outT"].T for b in range(NCORE)], axis=0)
    return np.ascontiguousarray(out.astype(np.float32))
```
